# Optimizing a Trainium2 kernel written in Bass

```python
import math
import jax, jax.numpy as jnp
from jax import lax
import numpy as np

D_MODEL = 1024
BATCH = 16
SEQ = 4096
DEPTH = 1

CTX_LEN = 256
GRID_W = 64
D_SSM = D_MODEL // 2
SSM_GROUP = 16
N_SSM_GROUPS = D_SSM // SSM_GROUP
SSM_STATE = 64
D_CONV = D_MODEL // 2
N_EXPERTS = 32
TOP_K = 4
D_EXPERT = D_MODEL
SWIGLU_LIMIT = 7.0
SWIGLU_ALPHA = 1.702
MOE_BLOCK = 128
RMS_EPS = 1e-6
D_IN = D_SSM + 3 * D_CONV + 2 * D_MODEL
SPLITS = (D_SSM, D_SSM + D_CONV, D_SSM + 2 * D_CONV, D_SSM + 3 * D_CONV,
          D_SSM + 3 * D_CONV + D_MODEL)

kernel_name = "hybrid_s5_shortconv_moe_dit_block"


def rmsnorm(x, g):
    xf = x.astype(jnp.float32)
    y = xf * lax.rsqrt(jnp.mean(xf * xf, axis=-1, keepdims=True) + RMS_EPS)
    return (y * g.astype(jnp.float32)).astype(x.dtype)


def adaln(cond, w_mod, b_mod):
    m = jax.nn.silu(cond) @ w_mod + b_mod
    return jnp.split(m, 6, axis=-1)


def modulate(h, shift, scale):
    return h * (1 + scale) + shift


def s5_discretise(lam_re, lam_im, log_dt, b_re, b_im):
    f = jnp.float32
    lam_re, lam_im = lam_re.astype(f), lam_im.astype(f)
    b_re, b_im = b_re.astype(f), b_im.astype(f)
    dt = jnp.exp(log_dt.astype(f))[:, None]
    mag = jnp.exp(lam_re * dt)
    a_re, a_im = mag * jnp.cos(lam_im * dt), mag * jnp.sin(lam_im * dt)
    den = lam_re * lam_re + lam_im * lam_im
    q_re = ((a_re - 1) * lam_re + a_im * lam_im) / den
    q_im = (a_im * lam_re - (a_re - 1) * lam_im) / den
    bb_re = q_re[..., None] * b_re - q_im[..., None] * b_im
    bb_im = q_re[..., None] * b_im + q_im[..., None] * b_re
    return a_re, a_im, bb_re, bb_im


def _complex_affine_combine(e1, e2):
    a1r, a1i, b1r, b1i = e1
    a2r, a2i, b2r, b2i = e2
    return (a1r * a2r - a1i * a2i, a1r * a2i + a1i * a2r,
            a2r * b1r - a2i * b1i + b2r, a2r * b1i + a2i * b1r + b2i)


def s5_scan(u, disc, h0, reverse):
    a_re, a_im, bb_re, bb_im = disc
    bu_re = jnp.einsum("blgc,gpc->lbgp", u, bb_re)
    bu_im = jnp.einsum("blgc,gpc->lbgp", u, bb_im)
    if h0 is not None:
        h0_re, h0_im = h0
        entry = -1 if reverse else 0
        bu_re = bu_re.at[entry].add(a_re * h0_re - a_im * h0_im)
        bu_im = bu_im.at[entry].add(a_re * h0_im + a_im * h0_re)
    seq_len = u.shape[1]
    ar = jnp.broadcast_to(a_re[None, None], (seq_len, 1) + a_re.shape)
    ai = jnp.broadcast_to(a_im[None, None], (seq_len, 1) + a_im.shape)
    _, _, h_re, h_im = lax.associative_scan(
        _complex_affine_combine, (ar, ai, bu_re, bu_im), reverse=reverse, axis=0)
    return h_re, h_im


def s5_readout(states, c_re, c_im):
    h_re, h_im = states
    return (jnp.einsum("lbgp,gcp->blgc", h_re, c_re.astype(jnp.float32))
            - jnp.einsum("lbgp,gcp->blgc", h_im, c_im.astype(jnp.float32)))


def s5_output(u_flat, s_f, s_b, c_re, c_im, d_skip, w_glu):
    bsz, seq_len, _ = u_flat.shape
    uf = u_flat.astype(jnp.float32)
    y = s5_readout(s_f, c_re[0], c_im[0]) + s5_readout(s_b, c_re[1], c_im[1])
    y = y.reshape(bsz, seq_len, D_SSM) + d_skip.astype(jnp.float32) * uf
    y = jax.nn.gelu(y)
    y = y * jax.nn.sigmoid(y @ w_glu.astype(jnp.float32))
    return y.astype(u_flat.dtype)


def short_conv(v, gate_b, gate_c, conv_w, conv_b, rows, row_len):
    bsz, seq_len, ch = v.shape
    z = (gate_c * v).reshape(bsz, rows, row_len, ch)
    zp = jnp.pad(z, ((0, 0), (0, 0), (1, 1), (0, 0)))
    y = (zp[:, :, :-2] * conv_w[0] + zp[:, :, 1:-1] * conv_w[1]
         + zp[:, :, 2:] * conv_w[2] + conv_b)
    return gate_b * y.reshape(bsz, seq_len, ch)


def mixer_merge(parts, s_f, s_b, rows, row_len, c_re, c_im, d_skip, w_glu,
                conv_w, conv_b, w_ssm_br, w_conv_br, w_o):
    u, v, gb, gc, g_s, g_c = parts
    y_s = s5_output(u, s_f, s_b, c_re, c_im, d_skip, w_glu) @ w_ssm_br
    y_c = short_conv(v, gb, gc, conv_w, conv_b, rows, row_len) @ w_conv_br
    return (jax.nn.sigmoid(g_s) * y_s + jax.nn.sigmoid(g_c) * y_c) @ w_o


def group_channels(u):
    bsz, seq_len, _ = u.shape
    return u.astype(jnp.float32).reshape(bsz, seq_len, N_SSM_GROUPS, SSM_GROUP)


def moe_ffn(h, w_router, b_router, w_gu, b_gu, w_down, b_down):
    bsz, seq_len, d = h.shape
    t = h.reshape(-1, d)
    n_tok = t.shape[0]
    n_slot = n_tok * TOP_K
    logits = (t @ w_router + b_router).astype(jnp.float32)
    top_logit, top_expert = lax.top_k(logits, TOP_K)
    gate = jax.nn.softmax(top_logit, axis=-1)
    slot_expert = top_expert.reshape(-1)
    order = jnp.argsort(slot_expert)
    srt_expert = slot_expert[order]
    srt_token = order // TOP_K
    srt_gate = gate.reshape(-1)[order]
    counts = jnp.bincount(slot_expert, length=N_EXPERTS)
    padded = (counts + MOE_BLOCK - 1) // MOE_BLOCK * MOE_BLOCK
    pad_end = jnp.cumsum(padded)
    pad_start = pad_end - padded
    start = jnp.cumsum(counts) - counts
    dest = pad_start[srt_expert] + jnp.arange(n_slot) - start[srt_expert]
    n_blocks = -(-n_slot // MOE_BLOCK) + N_EXPERTS
    cap = n_blocks * MOE_BLOCK
    buf_token = jnp.full((cap,), n_tok, jnp.int32).at[dest].set(srt_token.astype(jnp.int32))
    buf_gate = jnp.zeros((cap,), jnp.float32).at[dest].set(srt_gate)
    block_expert = jnp.minimum(
        jnp.searchsorted(pad_end, jnp.arange(n_blocks) * MOE_BLOCK, side="right"),
        N_EXPERTS - 1)
    t_pad = jnp.concatenate([t, jnp.zeros((1, d), t.dtype)], axis=0)
    xb = t_pad[buf_token].reshape(n_blocks, MOE_BLOCK, d)

    def expert_block(args):
        xe, e = args
        gu = xe @ w_gu[e] + b_gu[e]
        g, up = gu[:, :D_EXPERT], gu[:, D_EXPERT:]
        g = jnp.minimum(g, SWIGLU_LIMIT)
        up = jnp.clip(up, -SWIGLU_LIMIT, SWIGLU_LIMIT)
        act = g * jax.nn.sigmoid(SWIGLU_ALPHA * g) * (up + 1)
        return act @ w_down[e] + b_down[e]

    yb = lax.map(expert_block, (xb, block_expert)).reshape(cap, d)
    yb = yb * buf_gate[:, None].astype(yb.dtype)
    out = jnp.zeros((n_tok + 1, d), yb.dtype).at[buf_token].add(yb)[:n_tok]
    return out.reshape(bsz, seq_len, d)


def setup_inputs(seed: int = 0) -> dict:
    key = jax.random.key(seed)
    ks = iter(jax.random.split(key, 40))
    f = jnp.float32

    def nrm(shape, scale):
        return scale * jax.random.normal(next(ks), shape, f)

    G, P, CH = N_SSM_GROUPS, SSM_STATE, SSM_GROUP
    n_idx = jnp.arange(P, dtype=f)
    lam_re = -0.5 + nrm((DEPTH, 2, G, P), 0.01)
    lam_im = math.pi * n_idx + nrm((DEPTH, 2, G, P), 0.01)
    log_dt = jax.random.uniform(next(ks), (DEPTH, 2, G), f, math.log(1e-3), math.log(1e-1))
    return {
        "x": nrm((BATCH, SEQ, D_MODEL), 1.0),
        "c": nrm((BATCH, D_MODEL), 1.0),
        "ctx": nrm((BATCH, CTX_LEN, D_MODEL), 1.0),
        "c_ctx": nrm((D_MODEL,), 1.0),
        "w_mod": nrm((DEPTH, D_MODEL, 6 * D_MODEL), D_MODEL ** -0.5),
        "b_mod": nrm((DEPTH, 6 * D_MODEL), 0.02),
        "g_mix": 1.0 + nrm((DEPTH, D_MODEL), 0.02),
        "w_in": nrm((DEPTH, D_MODEL, D_IN), D_MODEL ** -0.5),
        "lam_re": lam_re,
        "lam_im": lam_im,
        "log_dt": log_dt,
        "b_re": nrm((DEPTH, 2, G, P, CH), (2 * CH) ** -0.5),
        "b_im": nrm((DEPTH, 2, G, P, CH), (2 * CH) ** -0.5),
        "c_re": nrm((DEPTH, 2, G, CH, P), (2 * P) ** -0.5),
        "c_im": nrm((DEPTH, 2, G, CH, P), (2 * P) ** -0.5),
        "d_skip": nrm((DEPTH, D_SSM), 1.0),
        "w_glu": nrm((DEPTH, D_SSM, D_SSM), D_SSM ** -0.5),
        "conv_w": nrm((DEPTH, 3, D_CONV), 3 ** -0.5),
        "conv_b": nrm((DEPTH, D_CONV), 0.02),
        "w_ssm_br": nrm((DEPTH, D_SSM, D_MODEL), D_SSM ** -0.5),
        "w_conv_br": nrm((DEPTH, D_CONV, D_MODEL), D_CONV ** -0.5),
        "w_o": nrm((DEPTH, D_MODEL, D_MODEL), D_MODEL ** -0.5),
        "g_ffn": 1.0 + nrm((DEPTH, D_MODEL), 0.02),
        "w_router": nrm((DEPTH, D_MODEL, N_EXPERTS), D_MODEL ** -0.5),
        "b_router": nrm((DEPTH, N_EXPERTS), 0.01),
        "w_gu": nrm((DEPTH, N_EXPERTS, D_MODEL, 2 * D_EXPERT), D_MODEL ** -0.5),
        "b_gu": nrm((DEPTH, N_EXPERTS, 2 * D_EXPERT), 0.02),
        "w_down": nrm((DEPTH, N_EXPERTS, D_EXPERT, D_MODEL), D_EXPERT ** -0.5),
        "b_down": nrm((DEPTH, N_EXPERTS, D_MODEL), 0.02),
        "g_final": 1.0 + nrm((D_MODEL,), 0.02),
    }


def reference(x, c, ctx, c_ctx, w_mod, b_mod, g_mix, w_in, lam_re, lam_im, log_dt,
              b_re, b_im, c_re, c_im, d_skip, w_glu, conv_w, conv_b, w_ssm_br,
              w_conv_br, w_o, g_ffn, w_router, b_router, w_gu, b_gu, w_down, b_down,
              g_final):
    rows = x.shape[1] // GRID_W
    ctx_len = ctx.shape[1]
    for i in range(DEPTH):
        last = i == DEPTH - 1
        mx = adaln(c[:, None, :], w_mod[i], b_mod[i])
        mc = adaln(c_ctx[None, None, :], w_mod[i], b_mod[i])

        hx = modulate(rmsnorm(x, g_mix[i]), mx[0], mx[1])
        hc = modulate(rmsnorm(ctx, g_mix[i]), mc[0], mc[1])
        px = jnp.split(hx @ w_in[i], SPLITS, axis=-1)
        pc = jnp.split(hc @ w_in[i], SPLITS, axis=-1)
        disc_f = s5_discretise(lam_re[i, 0], lam_im[i, 0], log_dt[i, 0], b_re[i, 0], b_im[i, 0])
        disc_b = s5_discretise(lam_re[i, 1], lam_im[i, 1], log_dt[i, 1], b_re[i, 1], b_im[i, 1])
        uc = group_channels(pc[0])
        ux = group_channels(px[0])
        sc_f = s5_scan(uc, disc_f, None, False)
        sc_b = s5_scan(uc, disc_b, None, True)
        h0_f = (sc_f[0][-1], sc_f[1][-1])
        h0_b = (sc_b[0][0], sc_b[1][0])
        sx_f = s5_scan(ux, disc_f, h0_f, False)
        sx_b = s5_scan(ux, disc_b, h0_b, True)
        mix_params = (c_re[i], c_im[i], d_skip[i], w_glu[i], conv_w[i], conv_b[i],
                      w_ssm_br[i], w_conv_br[i], w_o[i])
        x = x + mx[2] * mixer_merge(px, sx_f, sx_b, rows, GRID_W, *mix_params)
        if not last:
            ctx = ctx + mc[2] * mixer_merge(pc, sc_f, sc_b, 1, ctx_len, *mix_params)

        moe_params = (w_router[i], b_router[i], w_gu[i], b_gu[i], w_down[i], b_down[i])
        x = x + mx[5] * moe_ffn(modulate(rmsnorm(x, g_ffn[i]), mx[3], mx[4]), *moe_params)
        if not last:
            ctx = ctx + mc[5] * moe_ffn(modulate(rmsnorm(ctx, g_ffn[i]), mc[3], mc[4]), *moe_params)
    return rmsnorm(x, g_final)
```

```python
import math
import numpy as np
import concourse.bass as bass
import concourse.mybir as mybir
from concourse.bass_utils import run_bass_kernel_spmd

F32 = mybir.dt.float32
BF16 = mybir.dt.bfloat16
I32 = mybir.dt.int32
ALU = mybir.AluOpType
AF = mybir.ActivationFunctionType
POOL_ENG = mybir.EngineType.Pool

NCORES = 8
D = 1024
L = 4096
NBL = 2
NTOK = NBL * L
CTXL = 256
NE = 32
TOPK = 4
NSLOT = NTOK * TOPK
NBLK = NSLOT // 128 + NE
CAP = NBLK * 128
TWO_PI_LO = 6.2831845
SBUF_BASE = 16384
SBUF_LIMIT = SBUF_BASE + 206 * 1024
DT_SIZE = {F32: 4, BF16: 2, I32: 4}


class Buf:
    __slots__ = ("name", "lw", "rd")

    def __init__(self, name):
        self.name = name
        self.lw = None
        self.rd = []


class Tl:
    def __init__(self, h, name):
        self.h = h
        self.b = Buf(name)
        self.name = name
        self._sub = {}

    def __getitem__(self, k):
        return self.h[k]

    def sub(self, key):
        s = self._sub.get(key)
        if s is None:
            s = self._sub[key] = Buf("%s/%s" % (self.name, key))
        return s


def _bufs(xs):
    out = []
    for x in xs:
        if x is None:
            continue
        out.append(x.b if isinstance(x, Tl) else x)
    return out


class Sched:
    NDMA = 32

    def __init__(self, nc):
        self.nc = nc
        self.ops = []
        self.engs = {"pe": nc.tensor, "act": nc.scalar, "dve": nc.vector, "pool": nc.gpsimd, "sp": nc.sync}
        self.sems = {k: nc.alloc_semaphore("s_" + k) for k in ("pe", "act", "dve", "pool")}
        self.dsems = [nc.alloc_semaphore("d%d" % i) for i in range(self.NDMA)]

    def op(self, eng, fn, reads=(), writes=(), dma=False):
        self.ops.append((eng, fn, _bufs(reads), _bufs(writes), dma, False))
        if getattr(self, "tag_next", None):
            self.tags = getattr(self, "tags", {})
            self.tags[len(self.ops) - 1] = self.tag_next
            self.tag_next = None

    def barrier(self, fn):
        self.ops.append(("pool", fn, [], [], False, True))

    def finalize(self):
        ops = self.ops
        n = len(ops)
        deps = [None] * n
        needed = [False] * n
        dslot_last = [None] * self.NDMA
        dcount = [0] * self.NDMA
        dinfo = {}
        dk = 0
        last_on = {}
        last_bar = None
        for i, (eng, fn, reads, writes, dma, isbar) in enumerate(ops):
            d = set()
            if isbar:
                d.update(last_on.values())
                d.update(x for x in dslot_last if x is not None)
                last_bar = i
            else:
                if last_bar is not None:
                    d.add(last_bar)
                for b in reads:
                    if b.lw is not None:
                        d.add(b.lw)
                for b in writes:
                    if b.lw is not None:
                        d.add(b.lw)
                    if isinstance(b.rd, dict):
                        for v_ in b.rd.values():
                            if isinstance(v_, list):
                                d.update(v_)
                            else:
                                d.add(v_)
                rkey = "dma" if dma else eng
                for b in reads:
                    if not isinstance(b.rd, dict):
                        b.rd = {}
                    if dma:
                        b.rd.setdefault("dma", []).append(i)
                    else:
                        b.rd[rkey] = i
                for b in writes:
                    b.lw = i
                    b.rd = {}
            if dma:
                k = dk % self.NDMA
                dk += 1
                if dslot_last[k] is not None:
                    d.add(dslot_last[k])
                dslot_last[k] = i
                dcount[k] += 1
                dinfo[i] = (k, dcount[k] * 16)
            else:
                last_on[eng] = i
            d.discard(i)
            if eng == "pe" and not dma:
                d = {j for j in d if not (ops[j][0] == "pe" and not ops[j][4])}
            deps[i] = d
            for j in d:
                needed[j] = True
        cnt = {k: 0 for k in self.sems}
        comp = [None] * n
        for i, (eng, fn, reads, writes, dma, isbar) in enumerate(ops):
            if dma:
                k, v = dinfo[i]
                comp[i] = (("d", k), v)
            elif needed[i]:
                cnt[eng] += 1
                comp[i] = ((eng,), cnt[eng])
        known = {e: {} for e in self.engs}
        nwait = 0
        for i, (eng, fn, reads, writes, dma, isbar) in enumerate(ops):
            e = self.engs[eng]
            kn = known[eng]
            need = {}
            for j in deps[i]:
                key, v = comp[j]
                if kn.get(key, 0) < v:
                    need[key] = max(need.get(key, 0), v)
            if i in getattr(self, "tags", {}):
                print("TAG", self.tags[i], "op", i, eng, "needs", need, "deps", sorted((j, ops[j][0], comp[j]) for j in deps[i]))
            for key, v in need.items():
                sem = self.dsems[key[1]] if key[0] == "d" else self.sems[key[0]]
                e.wait_ge(sem, v)
                kn[key] = v
                nwait += 1
            inst = fn(e)
            if dma:
                k, v = dinfo[i]
                inst.then_inc(self.dsems[k], 16)
            elif needed[i]:
                inst.then_inc(self.sems[eng], 1)
        sp = self.engs["sp"]
        for k in range(self.NDMA):
            if dcount[k]:
                sp.wait_ge(self.dsems[k], dcount[k] * 16)
        for k, v in cnt.items():
            if v:
                sp.wait_ge(self.sems[k], v)
        self.stats = dict(ops=n, waits=nwait, cnt=cnt)


class Arena:
    def __init__(self, nc):
        self.nc = nc
        self.top = SBUF_BASE
        self.n = 0
        self.peak = 0

    def tile(self, name, shape, dtype):
        per = 1
        for s in shape[1:]:
            per *= s
        nbytes = per * DT_SIZE[dtype]
        off = (self.top + 63) // 64 * 64
        assert off + nbytes <= SBUF_LIMIT, ("SBUF overflow", name, off, nbytes)
        self.top = off + nbytes
        self.peak = max(self.peak, self.top)
        self.n += 1
        h = self.nc.alloc_sbuf_tensor_at("%s_%d" % (name, self.n), list(shape), dtype, offset=off)
        return Tl(h, name)

    def ring(self, name, shape, dtype, n):
        return Ring([self.tile("%s%d" % (name, i), shape, dtype) for i in range(n)])

    def mark(self):
        return self.top

    def release(self, m):
        self.top = m


class Ring:
    def __init__(self, tiles):
        self.t = tiles
        self.i = 0

    def next(self):
        t = self.t[self.i % len(self.t)]
        self.i += 1
        return t


def bc_ap(ap, shape):
    return ap.to_broadcast(list(shape))


def build_program(dbg=None):
    nc = bass.Bass("TRN2", target_bir_lowering=False)
    S = Sched(nc)
    A = Arena(nc)
    dbg = dbg or {}

    def din(name, shape, dt=F32):
        return nc.dram_tensor(name, list(shape), dt, kind="ExternalInput").ap()

    x_d = din("x", [NTOK, D])
    cond_d = din("cond3", [3, D])
    ctx_d = din("ctx", [NBL * CTXL, D])
    w_mod_d = din("w_mod", [D, 6 * D])
    b_mod_d = din("b_mod", [6 * D])
    g_mix_d = din("g_mix", [D])
    w_in_d = din("w_in", [D, 4096])
    lam_re_d = din("lam_re", [64, 64])
    lam_im_d = din("lam_im", [64, 64])
    log_dt_d = din("log_dt", [1, 64])
    b_re_d = din("b_re", [2, 32, 64, 16])
    b_im_d = din("b_im", [2, 32, 64, 16])
    c_re_d = din("c_re", [8, 8, 16, 64])
    c_im_d = din("c_im", [8, 8, 16, 64])
    d_skip_d = din("d_skip", [512])
    w_glu_d = din("w_glu", [512, 512])
    conv_w_d = din("conv_w", [3, 512])
    conv_b_d = din("conv_b", [512])
    w_ssm_br_d = din("w_ssm_br", [512, D])
    w_conv_br_d = din("w_conv_br", [512, D])
    w_o_d = din("w_o", [D, D])
    g_ffn_d = din("g_ffn", [D])
    if dbg.get("stage", 99) < 5:
        din_moe = lambda *a, **k: None
    else:
        din_moe = din
    w_router_d = din_moe("w_router", [D, NE])
    b_router_d = din_moe("b_router", [1, NE])
    w_gu_d = din_moe("w_gu", [NE, D, 2 * D])
    b_gu_d = din_moe("b_gu", [NE, 2 * D])
    w_down_d = din_moe("w_down", [NE, D, D])
    b_down_d = din_moe("b_down", [NE, D])
    g_final_d = din("g_final", [D])
    out_d = nc.dram_tensor("out", [NTOK, D], F32, kind="ExternalOutput").ap()

    def dscr(name, shape, dt):
        return Tl(nc.dram_tensor(name, list(shape), dt, kind="Internal").ap(), name)

    x1_s = dscr("x1_s", [NTOK, D], F32)
    hb_s = dscr("hb_s", [NTOK, D], BF16)
    xb_s = dscr("xb_s", [CAP, D], BF16)
    yb_s = dscr("yb_s", [CAP, D], F32)

    wgu_s = dscr("wgu_s", [NE * 128, 8 * 2 * D], BF16)
    wdn_s = dscr("wdn_s", [NE * 128, 8 * D], BF16)
    conv_jobs = [(k_, e_) for e_ in range(NE) for k_ in (0, 1)] if dbg.get("stage", 99) >= 5 else []
    conv_state = [0]

    def conv_step(n):
        for _ in range(n):
            if conv_state[0] >= len(conv_jobs):
                return
            k_, e_ = conv_jobs[conv_state[0]]
            conv_state[0] += 1
            if k_ == 0:
                S.op("pool", lambda e, e_=e_: e.dma_start(out=wgu_s[e_ * 128:(e_ + 1) * 128, :].rearrange("p (kc n) -> p kc n", kc=8),
                                                        in_=w_gu_d[e_].rearrange("(kc p) n -> p kc n", p=128)), [], [wgu_s.sub(e_)], dma=True)
            else:
                S.op("pool", lambda e, e_=e_: e.dma_start(out=wdn_s[e_ * 128:(e_ + 1) * 128, :].rearrange("p (kc n) -> p kc n", kc=8),
                                                        in_=w_down_d[e_].rearrange("(kc p) n -> p kc n", p=128)), [], [wdn_s.sub(e_)], dma=True)

    dbg_out = {}

    def dbg_tensor(name, shape):
        t = nc.dram_tensor(name, list(shape), F32, kind="ExternalOutput").ap()
        dbg_out[name] = t
        return t

    banks = []
    for i in range(8):
        banks.append(Tl(nc.alloc_psum_tensor("bank%d" % i, [128, 512], F32), "bank%d" % i))
    bank_rr = Ring(banks)

    def dma(eng, out, in_, R, W, **kw):
        S.op(eng, lambda e: e.dma_start(out=out, in_=in_, **kw), R, W, dma=True)

    def tt(eng, out, in0, in1, op, R, W):
        S.op(eng, lambda e: e.tensor_tensor(out=out, in0=in0, in1=in1, op=op), R, W)

    def ts(eng, out, in0, s1, s2, op0, op1, R, W):
        if s2 is None:
            S.op(eng, lambda e: e.tensor_scalar(out=out, in0=in0, scalar1=s1, scalar2=None, op0=op0), R, W)
        else:
            S.op(eng, lambda e: e.tensor_scalar(out=out, in0=in0, scalar1=s1, scalar2=s2, op0=op0, op1=op1), R, W)

    def stt(out, in0, scalar, in1, op0, op1, R, W):
        S.op("dve", lambda e: e.scalar_tensor_tensor(out=out, in0=in0, scalar=scalar, in1=in1, op0=op0, op1=op1), R, W)

    def act(out, in_, func, R, W, scale=1.0, bias=None, accum=None):
        def f(e):
            kw = {}
            if bias is not None:
                kw["bias"] = bias
            if accum is not None:
                kw["accum_out"] = accum
            return e.activation(out=out, in_=in_, func=func, scale=scale, **kw)
        S.op("act", f, R, W)

    def cp(eng, out, in_, R, W):
        if eng == "act":
            S.op("act", lambda e: e.copy(out=out, in_=in_), R, W)
        else:
            S.op(eng, lambda e: e.tensor_copy(out=out, in_=in_), R, W)

    def mm(out, lhsT, rhs, start, stop, R, W):
        S.op("pe", lambda e: e.matmul(out, lhsT=lhsT, rhs=rhs, start=start, stop=stop), R, W)

    def tr(out, in_, ident, R, W):
        S.op("pe", lambda e: e.transpose(out=out, in_=in_, identity=ident), R, W)

    def memset(eng, ap, val, W):
        S.op(eng, lambda e: e.memset(ap, val), [], W)

    def barrier():
        S.barrier(lambda e: e.memset(bar_t[:], 0.0))

    bar_t = A.tile("bar", [128, 1], F32)
    identf = A.tile("identf", [128, 128], F32)
    identb = A.tile("identb", [128, 128], BF16)
    onesf = A.tile("onesf", [128, 128], F32)
    pswap = A.tile("pswap", [128, 128], F32)
    sgn = A.tile("sgn", [128, 1], F32)
    nsgn = A.tile("nsgn", [128, 1], F32)
    neg1 = A.tile("neg1", [128, 1], F32)
    epsc = A.tile("epsc", [128, 1], F32)
    magc = A.tile("magc", [128, 4], F32)
    rowmask = A.tile("rowmask", [128, 8], F32)
    modT = A.tile("modT", [128, 48, 3], F32)
    gsm = A.tile("gsm", [128, 8, 3], F32)
    gsf = A.tile("gsf", [128, 8, 3], F32)
    gmT = A.tile("gmT", [128, 8], F32)
    gfT = A.tile("gfT", [128, 8], F32)
    gfinT = A.tile("gfinT", [128, 8], F32)
    dskT = A.tile("dskT", [128, 4], F32)
    cwT = A.tile("cwT", [128, 3, 4], F32)
    cbT = A.tile("cbT", [128, 4], F32)
    rcol = A.tile("rcol", [128, 64], F32)
    fcol = A.tile("fcol", [128, 64], F32)
    BB1T = A.tile("BB1T", [128, 8, 128], BF16)
    BB2T = A.tile("BB2T", [128, 8, 128], BF16)
    W1d = A.tile("W1d", [128, 8, 128], BF16)
    W2d = A.tile("W2d", [128, 8, 128], BF16)

    memset("pool", identf[:], 0.0, [identf])
    S.op("pool", lambda e: e.affine_select(out=identf[:], in_=identf[:], pattern=[[-1, 128]], compare_op=ALU.not_equal, fill=1.0, base=0, channel_multiplier=1), [identf], [identf])
    cp("dve", identb[:], identf[:], [identf], [identb])
    memset("pool", onesf[:], 1.0, [onesf])
    memset("pool", pswap[:], 0.0, [pswap])
    S.op("pool", lambda e: e.affine_select(out=pswap[:], in_=pswap[:], pattern=[[1, 128]], compare_op=ALU.not_equal, fill=1.0, base=-64, channel_multiplier=-1), [pswap], [pswap])
    S.op("pool", lambda e: e.affine_select(out=pswap[:], in_=pswap[:], pattern=[[1, 128]], compare_op=ALU.not_equal, fill=1.0, base=64, channel_multiplier=-1), [pswap], [pswap])
    memset("pool", sgn[:], 1.0, [sgn])
    memset("pool", sgn[0:64, :], -1.0, [sgn])
    memset("pool", nsgn[:], -1.0, [nsgn])
    memset("pool", nsgn[0:64, :], 1.0, [nsgn])
    memset("pool", neg1[:], -1.0, [neg1])
    memset("pool", epsc[:], 1e-6, [epsc])
    memset("pool", magc[:, 0:1], 12582912.0, [magc])
    memset("pool", magc[:, 1:2], -12582912.0, [magc])
    memset("pool", magc[:, 2:3], 0.25, [magc])
    memset("pool", magc[:, 3:4], 0.0, [magc])
    memset("pool", rowmask[:], 1.0, [rowmask])
    S.op("pool", lambda e: e.affine_select(out=rowmask[:], in_=rowmask[:], pattern=[[-16, 8]], compare_op=ALU.is_ge, fill=0.0, base=0, channel_multiplier=1), [rowmask], [rowmask])
    S.op("pool", lambda e: e.affine_select(out=rowmask[:], in_=rowmask[:], pattern=[[16, 8]], compare_op=ALU.is_ge, fill=0.0, base=15, channel_multiplier=-1), [rowmask], [rowmask])

    def load_cols(tile_, ap_slice, src_ap):
        dma("sp", ap_slice, src_ap.rearrange("(c p) -> p c", p=128), [], [tile_], allow_slow_non_contiguous=True)

    load_cols(gmT, gmT[:], g_mix_d)
    load_cols(gfT, gfT[:], g_ffn_d)
    load_cols(gfinT, gfinT[:], g_final_d)
    load_cols(dskT, dskT[:], d_skip_d)
    load_cols(cbT, cbT[:], conv_b_d)
    for k in range(3):
        load_cols(cwT, cwT[:, k, :], conv_w_d[k])

    m_setup = A.mark()
    condT = A.tile("condT", [128, 8, 3], F32)
    scT = A.tile("scT", [128, 8, 3], F32)
    bmT = A.tile("bmT", [128, 48], F32)
    modacc = A.tile("modacc", [128, 48, 3], F32)
    wm = A.ring("wm", [128, 6144], F32, 2)
    for b_ in range(3):
        dma("sp", condT[:, :, b_], cond_d[b_].rearrange("(kc p) -> p kc", p=128), [], [condT], allow_slow_non_contiguous=True)
    load_cols(bmT, bmT[:], b_mod_d)
    act(scT[:], condT[:], AF.Silu, [condT], [scT])
    for kc in range(8):
        w = wm.next()
        dma("sp", w[:], w_mod_d[kc * 128:(kc + 1) * 128, :], [], [w])
        pm = bank_rr.next()
        for jc in range(48):
            mm(pm[:, jc * 3:(jc + 1) * 3], w[:, jc * 128:(jc + 1) * 128], scT[:, kc, :], True, True, [w, scT], [pm])
        if kc == 0:
            cp("dve", modacc[:].rearrange("p a b -> p (a b)"), pm[:, 0:144], [pm], [modacc])
        else:
            tt("dve", modacc[:].rearrange("p a b -> p (a b)"), modacc[:].rearrange("p a b -> p (a b)"), pm[:, 0:144], ALU.add, [pm, modacc], [modacc])
    tt("dve", modT[:], modacc[:], bmT[:].rearrange("p (a o) -> p a o", o=1).to_broadcast([128, 48, 3]), ALU.add, [modacc, bmT], [modT])
    ts("dve", gsm[:], modT[:, 8:16, :], 1.0, None, ALU.add, None, [modT], [gsm])
    tt("dve", gsm[:], gsm[:], gmT[:].rearrange("p (a o) -> p a o", o=1).to_broadcast([128, 8, 3]), ALU.mult, [gsm, gmT], [gsm])
    ts("dve", gsf[:], modT[:, 32:40, :], 1.0, None, ALU.add, None, [modT], [gsf])
    tt("dve", gsf[:], gsf[:], gfT[:].rearrange("p (a o) -> p a o", o=1).to_broadcast([128, 8, 3]), ALU.mult, [gsf, gfT], [gsf])

    if "mod" in dbg:
        o = dbg_tensor("dbg_mod", [128, 48 * 3])
        dma("sp", o, modT[:].rearrange("p a b -> p (a b)"), [modT], [])

    lamn = A.tile("lamn", [64, 2, 128], F32)
    dma("sp", lamn[:, 0, 0:64], lam_re_d, [], [lamn])
    dma("sp", lamn[:, 0, 64:128], lam_re_d, [], [lamn])
    dma("sp", lamn[:, 1, 0:64], lam_im_d, [], [lamn])
    dma("sp", lamn[:, 1, 64:128], lam_im_d, [], [lamn])
    lreT = A.tile("lreT", [128, 64], F32)
    limT = A.tile("limT", [128, 64], F32)
    pl = bank_rr.next()
    tr(pl[:, 0:64], lamn[:, 0, :], identf[0:64, 0:64], [lamn, identf], [pl])
    tr(pl[:, 64:128], lamn[:, 1, :], identf[0:64, 0:64], [lamn, identf], [pl])
    cp("dve", lreT[:], pl[:, 0:64], [pl], [lreT])
    cp("dve", limT[:], pl[:, 64:128], [pl], [limT])
    dtb = A.tile("dtb", [128, 64], F32)
    dma("sp", dtb[:], bass.AP(tensor=log_dt_d.tensor, offset=0, ap=[[0, 128], [1, 64]]), [], [dtb])
    act(dtb[:], dtb[:], AF.Exp, [dtb], [dtb])
    s5 = {k: A.tile("s5" + k, [128, 64], F32) for k in "ang lrd t ks fs sn t2 fc cs are aim den am1 qre qim tmp tmp2 sq nq".split()}
    s5i = A.tile("s5i", [128, 64], I32)
    tt("dve", s5["ang"][:], limT[:], dtb[:], ALU.mult, [limT, dtb], [s5["ang"]])
    tt("dve", s5["lrd"][:], lreT[:], dtb[:], ALU.mult, [lreT, dtb], [s5["lrd"]])
    act(rcol[:], s5["lrd"][:], AF.Exp, [s5["lrd"]], [rcol])
    ts("dve", fcol[:], s5["ang"][:], 1.0 / (2 * math.pi), None, ALU.mult, None, [s5["ang"]], [fcol])
    cp("pool", s5i[:], fcol[:], [fcol], [s5i])
    tt("pool", s5["fs"][:], fcol[:], s5i[:], ALU.subtract, [fcol, s5i], [s5["fs"]])
    act(s5["sn"][:], s5["fs"][:], AF.Sin, [s5["fs"]], [s5["sn"]], scale=TWO_PI_LO)
    ts("pool", s5["t2"][:], fcol[:], 0.25, None, ALU.add, None, [fcol], [s5["t2"]])
    cp("pool", s5i[:], s5["t2"][:], [s5["t2"], s5["fs"]], [s5i])
    tt("pool", s5["fc"][:], s5["t2"][:], s5i[:], ALU.subtract, [s5["t2"], s5i], [s5["fc"]])
    act(s5["cs"][:], s5["fc"][:], AF.Sin, [s5["fc"]], [s5["cs"]], scale=TWO_PI_LO)
    tt("dve", s5["are"][:], rcol[:], s5["cs"][:], ALU.mult, [rcol, s5["cs"]], [s5["are"]])
    tt("dve", s5["aim"][:], rcol[:], s5["sn"][:], ALU.mult, [rcol, s5["sn"]], [s5["aim"]])
    tt("dve", s5["den"][:], lreT[:], lreT[:], ALU.mult, [lreT], [s5["den"]])
    tt("dve", s5["tmp"][:], limT[:], limT[:], ALU.mult, [limT], [s5["tmp"]])
    tt("dve", s5["den"][:], s5["den"][:], s5["tmp"][:], ALU.add, [s5["den"], s5["tmp"]], [s5["den"]])
    S.op("dve", lambda e: e.reciprocal(out=s5["den"][:], in_=s5["den"][:]), [s5["den"]], [s5["den"]])
    ts("dve", s5["am1"][:], s5["are"][:], -1.0, None, ALU.add, None, [s5["are"]], [s5["am1"]])
    tt("dve", s5["tmp"][:], s5["am1"][:], lreT[:], ALU.mult, [s5["am1"], lreT], [s5["tmp"]])
    tt("dve", s5["tmp2"][:], s5["aim"][:], limT[:], ALU.mult, [s5["aim"], limT], [s5["tmp2"]])
    tt("dve", s5["tmp"][:], s5["tmp"][:], s5["tmp2"][:], ALU.add, [s5["tmp"], s5["tmp2"]], [s5["tmp"]])
    tt("dve", s5["qre"][:], s5["tmp"][:], s5["den"][:], ALU.mult, [s5["tmp"], s5["den"]], [s5["qre"]])
    tt("dve", s5["tmp"][:], s5["aim"][:], lreT[:], ALU.mult, [s5["aim"], lreT], [s5["tmp"]])
    tt("dve", s5["tmp2"][:], s5["am1"][:], limT[:], ALU.mult, [s5["am1"], limT], [s5["tmp2"]])
    tt("dve", s5["tmp"][:], s5["tmp"][:], s5["tmp2"][:], ALU.subtract, [s5["tmp"], s5["tmp2"]], [s5["tmp"]])
    tt("dve", s5["qim"][:], s5["tmp"][:], s5["den"][:], ALU.mult, [s5["tmp"], s5["den"]], [s5["qim"]])
    ts("dve", s5["sq"][:], s5["qim"][:], sgn[:, 0:1], None, ALU.mult, None, [s5["qim"], sgn], [s5["sq"]])
    ts("dve", s5["nq"][:], s5["qre"][:], nsgn[:, 0:1], None, ALU.mult, None, [s5["qre"], nsgn], [s5["nq"]])
    Ba = A.tile("Ba", [128, 64, 16], F32)
    Bb = A.tile("Bb", [128, 64, 16], F32)
    bsrc_re = b_re_d.rearrange("d g p c -> p (d g) c")
    bsrc_im = b_im_d.rearrange("d g p c -> p (d g) c")
    dma("sp", Ba[0:64], bsrc_re, [], [Ba])
    dma("sp", Ba[64:128], bsrc_im, [], [Ba])
    dma("sp", Bb[0:64], bsrc_im, [], [Bb])
    dma("sp", Bb[64:128], bsrc_re, [], [Bb])
    BB1 = A.tile("BB1", [128, 64, 16], F32)
    BB2 = A.tile("BB2", [128, 64, 16], F32)
    btmp = A.tile("btmp", [128, 64, 16], F32)

    def qb(name):
        return s5[name][:].rearrange("p (a o) -> p a o", o=1).to_broadcast([128, 64, 16])
    tt("dve", BB1[:], Ba[:], qb("qre"), ALU.mult, [Ba, s5["qre"]], [BB1])
    tt("dve", btmp[:], Bb[:], qb("sq"), ALU.mult, [Bb, s5["sq"]], [btmp])
    tt("dve", BB1[:], BB1[:], btmp[:], ALU.add, [BB1, btmp], [BB1])
    tt("dve", BB2[:], Bb[:], qb("nq"), ALU.mult, [Bb, s5["nq"]], [BB2])
    tt("dve", btmp[:], Ba[:], qb("qim"), ALU.mult, [Ba, s5["qim"], BB1], [btmp])
    tt("dve", BB2[:], BB2[:], btmp[:], ALU.add, [BB2, btmp], [BB2])
    for dcc in range(8):
        for (src, dst) in ((BB1, BB1T), (BB2, BB2T)):
            pb = bank_rr.next()
            tr(pb[:, 0:128], src[:, dcc * 8:(dcc + 1) * 8, :].rearrange("p a c -> p (a c)"), identf[:], [src, identf], [pb])
            cp("act", dst[:, dcc, :], pb[:, 0:128], [pb], [dst])
    Cn1 = A.tile("Cn1", [128, 8, 128], F32)
    Cn2 = A.tile("Cn2", [128, 8, 128], F32)
    csrc_re = c_re_d.rearrange("a gl c p -> (gl c) a p")
    csrc_im = c_im_d.rearrange("a gl c p -> (gl c) a p")
    dma("sp", Cn1[:, :, 0:64], csrc_re, [], [Cn1])
    dma("sp", Cn1[:, :, 64:128], csrc_im, [], [Cn1])
    dma("sp", Cn2[:, :, 0:64], csrc_im, [], [Cn2])
    dma("sp", Cn2[:, :, 64:128], csrc_re, [], [Cn2])
    for dcc in range(8):
        pb = bank_rr.next()
        tr(pb[:, 0:128], Cn1[:, dcc, :], identf[:], [Cn1, identf], [pb])
        ts("dve", W1d[:, dcc, :], pb[:, 0:128], nsgn[:, 0:1], None, ALU.mult, None, [pb, nsgn], [W1d])
        pb = bank_rr.next()
        tr(pb[:, 0:128], Cn2[:, dcc, :], identf[:], [Cn2, identf], [pb])
        ts("dve", W2d[:, dcc, :], pb[:, 0:128], neg1[:, 0:1], None, ALU.mult, None, [pb, neg1], [W2d])

    barrier()
    A.release(m_setup)

    yg_s = dscr("yg_s", [128, 4, L], BF16)
    AS = {}

    def norm_tiles(arena_tiles, row_ap_fn, ntile, bidx, hxT, gs_t, sh_lo):
        xt_r, xn_r, sq_r, ss_r = arena_tiles
        stt_ = [dict() for _ in range(ntile)]

        def nA(t_):
            st = stt_[t_]
            st["xt"] = xt = xt_r.next()
            dma("sp", xt[:], row_ap_fn(t_), [], [xt])
            st["ss"] = ss = ss_r.next()
            st["xn"] = xn = xn_r.next()
            memset("pool", ss[:], 0.0, [ss])
            act(xn[:], xt[:], AF.Square, [xt, ss], [xn, ss], accum=ss[:, 0:1])
            act(ss[:, 1:2], ss[:, 0:1], AF.Ln, [ss], [ss], scale=1.0 / D, bias=epsc[:, 0:1])
            act(ss[:, 2:3], ss[:, 1:2], AF.Exp, [ss], [ss], scale=-0.5)

        def nB(t_):
            st = stt_[t_]
            xt, xn, ss = st["xt"], st["xn"], st["ss"]
            ts("dve", xn[:], xt[:], ss[:, 2:3], None, ALU.mult, None, [xt, ss], [xn])
            st["pT"] = pT = bank_rr.next()
            pTb = pT.h.bitcast(BF16)
            for kc in range(8):
                tr(pTb[:, kc * 128:(kc + 1) * 128], xn[:, kc * 128:(kc + 1) * 128], identb[:], [xn, identb], [pT])

        def nC(t_):
            pT = stt_[t_]["pT"]
            pTb = pT.h.bitcast(BF16)
            for kc in range(8):
                dst = hxT[:, kc, t_ * 128:(t_ + 1) * 128]
                src = pTb[:, kc * 128:(kc + 1) * 128]
                if kc % 2 == 0:
                    act(dst, src, AF.Identity, [pT, gs_t, modT], [hxT], scale=gs_t[:, kc, bidx:bidx + 1], bias=modT[:, sh_lo + kc, bidx:bidx + 1])
                else:
                    ts("dve", dst, src, gs_t[:, kc, bidx:bidx + 1], modT[:, sh_lo + kc, bidx:bidx + 1], ALU.mult, ALU.add, [pT, gs_t, modT], [hxT])
        for step in range(ntile + 2):
            if step < ntile:
                nA(step)
            if 0 <= step - 1 < ntile:
                nB(step - 1)
            if 0 <= step - 2 < ntile:
                nC(step - 2)

    def phase_A(b):
        uT, uTc = AS["uT"], AS["uTc"]
        m = A.mark()
        w_in_u = A.tile("w_in_u", [128, 8, 512], BF16)
        dma("pool", w_in_u[:], w_in_d[:, 0:512].rearrange("(kc p) n -> p kc n", p=128), [], [w_in_u])
        tiles = (A.ring("xt", [128, D], F32, 3), A.ring("xn", [128, D], BF16, 3), None, A.ring("ss", [128, 4], F32, 4))
        hx_r = A.ring("hxT", [128, 8, 512], BF16, 2)
        groups = [("ctx", None)] + [("x", tg) for tg in range(8)]
        for kind, tg in groups:
            conv_step(2)
            hxT = hx_r.next()
            if kind == "ctx":
                norm_tiles(tiles, lambda t_: ctx_d[b * CTXL + t_ * 128: b * CTXL + (t_ + 1) * 128, :], 2, 2, hxT, gsm, 0)
                ntok = 256
            else:
                base = b * L + tg * 512
                norm_tiles(tiles, lambda t_: x_d[base + t_ * 128: base + (t_ + 1) * 128, :], 4, b, hxT, gsm, 0)
                ntok = 512
            for cc in range(4):
                pu = bank_rr.next()
                for kc in range(8):
                    mm(pu[:, 0:ntok], w_in_u[:, kc, cc * 128:(cc + 1) * 128], hxT[:, kc, 0:ntok], kc == 0, kc == 7, [w_in_u, hxT], [pu])
                if kind == "ctx":
                    cp("act" if cc % 2 else "dve", uTc[:, cc, :], pu[:, 0:256], [pu], [uTc.sub(cc)])
                else:
                    cp("act" if cc % 2 else "dve", uT[:, cc, tg * 512:(tg + 1) * 512], pu[:, 0:512], [pu], [uT.sub((cc, tg))])
        barrier()
        A.release(m)

    def phase_S(b):
        uT, uTc = AS["uT"], AS["uTc"]
        m = A.mark()
        jrow = A.tile("jrow", [128, 2, 512], F32)
        S.op("pool", lambda e: e.iota(jrow[:, 0, :], pattern=[[1, 512]], base=1, channel_multiplier=0, allow_small_or_imprecise_dtypes=True), [], [jrow])
        S.op("pool", lambda e: e.iota(jrow[:, 1, :], pattern=[[-1, 512]], base=512, channel_multiplier=0, allow_small_or_imprecise_dtypes=True), [], [jrow])
        COS = A.tile("COS", [128, 16, 512], F32)
        SIN = A.tile("SIN", [128, 16, 512], F32)
        ccol = A.tile("ccol", [128, 16, 2], F32)
        scol = A.tile("scol", [128, 16, 2], F32)
        X1w = A.tile("X1w", [128, 16, 128], BF16)
        X2w = A.tile("X2w", [128, 16, 128], BF16)
        W1p = A.tile("W1p", [128, 16, 128], BF16)
        W2p = A.tile("W2p", [128, 16, 128], BF16)
        tg_t = A.ring("tgt", [128, 512], F32, 3)
        tg_i = A.ring("tgi", [128, 512], F32, 3)
        m1_r = A.ring("m1", [128, 512], BF16, 4)
        x2_r = A.ring("x2s", [128, 512], BF16, 4)
        G_r = A.ring("G", [128, 512], F32, 4)
        G1_r = A.ring("G1", [128, 512], BF16, 4)
        G2_r = A.ring("G2", [128, 512], BF16, 4)
        carry = A.tile("carry", [128, 16], F32)
        ct1 = A.tile("ct1", [128, 16], F32)
        y_sb = A.tile("y_sb", [128, L], F32)
        ge = {k: A.ring("ge" + k, [128, 512], F32, n_) for k, n_ in (("y", 4), ("a", 2), ("b", 2), ("s", 2))}
        ybank = [banks[7], banks[7]]
        xbanks = Ring(banks[0:4])
        zbanks = Ring(banks[4:6])
        swbank = banks[6]
        for cc in range(4):
            tabs = []

            def mk_tab(d, gl, which, dst):
                u_ = d * 8 + gl
                dg = d * 32 + cc * 8 + gl
                h_ = {}

                def tA():
                    h_["t"] = t_ = tg_t.next()
                    r_ = tg_i.next()
                    act(t_[:], jrow[:, d, :], AF.Identity, [jrow, fcol, magc], [t_], scale=fcol[:, dg:dg + 1], bias=magc[:, 2:3] if which else magc[:, 3:4])
                    act(r_[:], t_[:], AF.Identity, [t_, magc], [r_], bias=magc[:, 0:1])
                    act(r_[:], r_[:], AF.Identity, [r_, magc], [r_], bias=magc[:, 1:2])
                    tt("pool", t_[:], t_[:], r_[:], ALU.subtract, [t_, r_], [t_])

                def tB():
                    act(dst[:, u_, :], h_["t"][:], AF.Sin, [h_["t"]], [dst.sub(u_)], scale=TWO_PI_LO)
                    if which == 1:
                        c512 = 511 if d == 0 else 0
                        c256 = 255 if d == 0 else 256
                        for k_, col in ((0, c512), (1, c256)):
                            cp("pool", ccol[:, u_, k_:k_ + 1], COS[:, u_, col:col + 1], [COS.sub(u_)], [ccol.sub(u_)])
                            ts("pool", scol[:, u_, k_:k_ + 1], SIN[:, u_, col:col + 1], sgn[:, 0:1], None, ALU.mult, None, [SIN.sub(u_), sgn], [scol.sub(u_)])
                return tA, tB

            for d in range(2):
                for gl in range(8):
                    u_ = d * 8 + gl
                    for which, dst in ((0, SIN), (1, COS)):
                        tabs.append(mk_tab(d, gl, which, dst))
                    dcc = d * 4 + cc
                    ts("dve", X1w[:, u_, :], BB1T[:, dcc, :], rowmask[:, gl:gl + 1], None, ALU.mult, None, [BB1T, rowmask], [X1w.sub(u_)])
                    ts("dve", X2w[:, u_, :], BB2T[:, dcc, :], rowmask[:, gl:gl + 1], None, ALU.mult, None, [BB2T, rowmask], [X2w.sub(u_)])
                    memset("pool", W1p[:, u_, :], 0.0, [W1p.sub(u_)])
                    memset("pool", W2p[:, u_, :], 0.0, [W2p.sub(u_)])
                    cp("pool", W1p[:, u_, gl * 16:(gl + 1) * 16], W1d[:, dcc, gl * 16:(gl + 1) * 16], [W1d], [W1p.sub(u_)])
                    cp("pool", W2p[:, u_, gl * 16:(gl + 1) * 16], W2d[:, dcc, gl * 16:(gl + 1) * 16], [W2d], [W2p.sub(u_)])
            for step in range(len(tabs) + 1):
                if step < len(tabs):
                    tabs[step][0]()
                if step >= 1:
                    tabs[step - 1][1]()

            def unit(d, gl, rhs_ap, rhs_bufs, n, tab_lo, init_ap, init_bufs, readout, last_col, kcol, ybk=None, first=False, last=False, post=None):
                u_ = d * 8 + gl
                dg = d * 32 + cc * 8 + gl
                st = {}
                cosv = COS[:, u_, tab_lo:tab_lo + n]
                sinv = SIN[:, u_, tab_lo:tab_lo + n]

                def s0():
                    st["p1"] = p1 = xbanks.next()
                    st["p2"] = p2 = xbanks.next()
                    mm(p1[:, 0:n], X1w[:, u_, :], rhs_ap, True, True, [X1w.sub(u_)] + rhs_bufs, [p1])
                    mm(p2[:, 0:n], X2w[:, u_, :], rhs_ap, True, True, [X2w.sub(u_)] + rhs_bufs, [p2])

                def s1():
                    st["m1"] = m1 = m1_r.next()
                    st["x2"] = x2 = x2_r.next()
                    tt("dve", m1[:, 0:n], st["p1"][:, 0:n], cosv, ALU.mult, [st["p1"], COS.sub(u_)], [m1])
                    tt("dve", x2[:, 0:n], st["p2"][:, 0:n], sinv, ALU.mult, [st["p2"], SIN.sub(u_)], [x2])

                def s2():
                    st["z"] = zb = zbanks.next()
                    mm(zb[:, 0:n], identb[:], st["m1"][:, 0:n], True, False, [identb, st["m1"]], [zb])
                    mm(zb[:, 0:n], identb[:], st["x2"][:, 0:n], False, True, [identb, st["x2"]], [zb])

                def s3():
                    st["G"] = G = G_r.next()
                    z = st["z"]
                    rbc = rcol[:, dg:dg + 1].to_broadcast([128, n])
                    if d == 0:
                        S.op("dve", lambda e: e.tensor_tensor_scan(out=G[:, 0:n], data0=rbc, data1=z[:, 0:n], initial=init_ap, op0=ALU.mult, op1=ALU.add), [z, rcol] + init_bufs, [G])
                    else:
                        S.op("dve", lambda e: e.tensor_tensor_scan(out=G[:, 0:n][:, ::-1], data0=rbc, data1=z[:, 0:n][:, ::-1], initial=init_ap, op0=ALU.mult, op1=ALU.add), [z, rcol] + init_bufs, [G])

                def s4():
                    G = st["G"]
                    mm(swbank[:, u_:u_ + 1], pswap[:], G[:, last_col:last_col + 1], True, True, [pswap, G], [swbank.sub(u_)])
                    act(ct1[:, u_:u_ + 1], swbank[:, u_:u_ + 1], AF.Identity, [swbank.sub(u_), scol.sub(u_)], [ct1.sub(u_)], scale=scol[:, u_, kcol:kcol + 1])
                    stt(carry[:, u_:u_ + 1], G[:, last_col:last_col + 1], ccol[:, u_, kcol:kcol + 1], ct1[:, u_:u_ + 1], ALU.mult, ALU.add, [G, ccol.sub(u_), ct1.sub(u_)], [carry.sub(u_)])
                    if readout:
                        st["G1"] = G1 = G1_r.next()
                        st["G2"] = G2 = G2_r.next()
                        tt("pool", G1[:, 0:n], G[:, 0:n], cosv, ALU.mult, [G, COS.sub(u_)], [G1])
                        tt("pool", G2[:, 0:n], G[:, 0:n], sinv, ALU.mult, [G, SIN.sub(u_)], [G2])

                def s5():
                    if readout:
                        mm(ybk[:, 0:n], W1p[:, u_, :], st["G1"][:, 0:n], first, False, [W1p.sub(u_), st["G1"]], [ybk])
                        mm(ybk[:, 0:n], W2p[:, u_, :], st["G2"][:, 0:n], False, last, [W2p.sub(u_), st["G2"]], [ybk])
                    if post is not None:
                        post()
                return [s0, s1, s2, s3, s4, s5]

            units = []
            for d in range(2):
                for gl in range(8):
                    lastc = 255 if d == 0 else 0
                    units.append(unit(d, gl, uTc[:, cc, :], [uTc.sub(cc)], 256, 0 if d == 0 else 256, 0.0, [], False, lastc, 1))
            touched = set()
            for i in range(8):
                for d in range(2):
                    tc = i if d == 0 else 7 - i
                    ybk = ybank[d]
                    for gl in range(8):
                        u_ = d * 8 + gl
                        lastc = 511 if d == 0 else 0
                        post = None
                        if gl == 7:
                            def post(tc=tc, ybk=ybk, fresh=(tc not in touched)):
                                ysl = y_sb[:, tc * 512:(tc + 1) * 512]
                                if fresh:
                                    cp("act", ysl, ybk[:], [ybk], [y_sb.sub(tc)])
                                else:
                                    tt("dve", ysl, ysl, ybk[:], ALU.add, [ybk, y_sb.sub(tc)], [y_sb.sub(tc)])
                            touched.add(tc)
                        units.append(unit(d, gl, uT[:, cc, tc * 512:(tc + 1) * 512], [uT.sub((cc, tc))], 512, 0, carry[:, u_:u_ + 1], [carry.sub(u_)], True, lastc, 0, ybk=ybk, first=(gl == 0), last=(gl == 7), post=post))
            NST = 6
            for step in range(len(units) + NST - 1):
                for k_ in range(NST):
                    ui = step - k_
                    if 0 <= ui < len(units):
                        units[ui][k_]()
            gst = [dict() for _ in range(8)]

            def gA(tc):
                sl = slice(tc * 512, (tc + 1) * 512)
                g_ = gst[tc]
                g_["y"] = yv = ge["y"].next()
                g_["a"] = a_ = ge["a"].next()
                stt(yv[:], uT[:, cc, sl], dskT[:, cc:cc + 1], y_sb[:, sl], ALU.mult, ALU.add, [uT.sub((cc, tc)), dskT, y_sb.sub(tc)], [yv])
                tt("dve", a_[:], yv[:], yv[:], ALU.mult, [yv], [a_])

            def gB(tc):
                g_ = gst[tc]
                a_, yv = g_["a"], g_["y"]
                g_["b"] = b_ = ge["b"].next()
                ts("pool", a_[:], a_[:], 0.044715, 1.0, ALU.mult, ALU.add, [a_], [a_])
                tt("pool", b_[:], a_[:], yv[:], ALU.mult, [a_, yv], [b_])

            def gC(tc):
                g_ = gst[tc]
                g_["s"] = s_ = ge["s"].next()
                act(s_[:], g_["b"][:], AF.Sigmoid, [g_["b"]], [s_], scale=1.5957691216057308)

            def gD(tc):
                sl = slice(tc * 512, (tc + 1) * 512)
                g_ = gst[tc]
                tt("dve", uT[:, cc, sl], g_["y"][:], g_["s"][:], ALU.mult, [g_["y"], g_["s"]], [uT.sub((cc, tc))])
            gfs = [gA, gB, gC, gD]
            for step in range(8 + 3):
                for k_ in range(4):
                    tc = step - k_
                    if 0 <= tc < 8:
                        gfs[k_](tc)
            for tc in range(8):
                dma("act", yg_s[:, cc, tc * 512:(tc + 1) * 512], uT[:, cc, tc * 512:(tc + 1) * 512], [uT.sub((cc, tc))], [yg_s.sub(tc)])
        barrier()
        A.release(m)

    def phase_B(b):
        m = A.mark()
        w_rest = A.tile("w_rest", [128, 8, 3584], BF16)
        w_glu = A.tile("w_glu", [128, 4, 512], BF16)
        w_sbr = A.tile("w_sbr", [128, 4, D], BF16)
        w_cbr = A.tile("w_cbr", [128, 4, D], BF16)
        w_o = A.tile("w_o", [128, 8, D], BF16)
        for kc in range(8):
            for h_ in range(2):
                lo = h_ * 1792
                dma("pool", w_rest[:, kc, lo:lo + 1792], w_in_d[kc * 128:(kc + 1) * 128, 512 + lo:512 + lo + 1792], [], [w_rest.sub(kc)])
        dma("pool", w_glu[:], w_glu_d.rearrange("(kc p) n -> p kc n", p=128), [], [w_glu])
        dma("pool", w_sbr[:], w_ssm_br_d.rearrange("(kc p) n -> p kc n", p=128), [], [w_sbr])
        dma("pool", w_cbr[:], w_conv_br_d.rearrange("(kc p) n -> p kc n", p=128), [], [w_cbr])
        dma("pool", w_o[:], w_o_d.rearrange("(kc p) n -> p kc n", p=128), [], [w_o])
        diag_r = A.ring("diag", [128, 128], F32, 2)
        for hh in range(2):
            pg = bank_rr.next()
            for q in range(4):
                j = hh * 4 + q
                dg_ = diag_r.next()
                ts("dve", dg_[:], identf[:], modT[:, 16 + j, b:b + 1], None, ALU.mult, None, [identf, modT], [dg_])
                mm(pg[:, q * 128:(q + 1) * 128], onesf[:], dg_[:], True, True, [onesf, dg_], [pg])
            for kc in range(8):
                tt("dve", w_o[:, kc, hh * 512:(hh + 1) * 512], w_o[:, kc, hh * 512:(hh + 1) * 512], pg[:], ALU.mult, [w_o, pg], [w_o])
        xt_ring = A.ring("xt", [128, D], F32, 4)
        tiles = (xt_ring, A.ring("xn", [128, D], BF16, 3), None, A.ring("ss", [128, 4], F32, 4))
        hx_r = A.ring("hxT", [128, 8, 512], BF16, 1)
        yg_r = A.ring("ygT", [128, 4, 512], BF16, 2)
        v_r = A.ring("v_sb", [128, 512], F32, 2)
        yc_r = A.ring("yc", [128, 512], F32, 2)
        convT = A.tile("convT", [128, 4, 512], BF16)
        ssmT = A.tile("ssmT", [128, 4, 512], BF16)
        sg_r = A.ring("sg", [128, 512], F32, 2)
        sgs_r = A.ring("sgs", [128, 512], F32, 2)
        sgc_r = A.ring("sgc", [128, 512], F32, 2)
        mgT = A.tile("mgT", [128, 8, 512], BF16)
        xo_r = xt_ring

        def wcol(c0):
            return c0 - 512

        for tg in range(8):
            base = b * L + tg * 512
            conv_step(2)
            hxT = hx_r.next()
            ygT = yg_r.next()
            dma("sp", ygT[:], yg_s[:, :, tg * 512:(tg + 1) * 512], [yg_s.sub(tg)], [ygT])
            norm_tiles(tiles, lambda t_: x_d[base + t_ * 128: base + (t_ + 1) * 128, :], 4, b, hxT, gsm, 0)

            def proj(c0):
                pb = bank_rr.next()
                for kc in range(8):
                    mm(pb[:], w_rest[:, kc, wcol(c0):wcol(c0) + 128], hxT[:, kc, :], kc == 0, kc == 7, [w_rest.sub(kc), hxT], [pb])
                return pb
            for cc in range(4):
                pv = proj(512 + cc * 128)
                pgb = proj(1024 + cc * 128)
                pgc = proj(1536 + cc * 128)
                v_sb = v_r.next()
                zc = v_sb
                yc = yc_r.next()
                cp("act", v_sb[:], pv[:], [pv], [v_sb])
                tt("dve", zc[:], pgc[:], v_sb[:], ALU.mult, [pgc, v_sb], [zc])
                ts("dve", yc[:], zc[:], cwT[:, 1, cc:cc + 1], cbT[:, cc:cc + 1], ALU.mult, ALU.add, [zc, cwT, cbT], [yc])
                z3 = zc[:].rearrange("p (r t) -> p r t", t=64)
                y3 = yc[:].rearrange("p (r t) -> p r t", t=64)
                stt(y3[:, :, 1:64], z3[:, :, 0:63], cwT[:, 0, cc:cc + 1], y3[:, :, 1:64], ALU.mult, ALU.add, [zc, cwT, yc], [yc])
                stt(y3[:, :, 0:63], z3[:, :, 1:64], cwT[:, 2, cc:cc + 1], y3[:, :, 0:63], ALU.mult, ALU.add, [zc, cwT, yc], [yc])
                tt("dve", convT[:, cc, :], pgb[:], yc[:], ALU.mult, [pgb, yc], [convT.sub(cc)])
            for oc in range(4):
                pb = bank_rr.next()
                for kc in range(4):
                    mm(pb[:], w_glu[:, kc, oc * 128:(oc + 1) * 128], ygT[:, kc, :], kc == 0, kc == 3, [w_glu, ygT], [pb])
                sg = sg_r.next()
                act(sg[:], pb[:], AF.Sigmoid, [pb], [sg])
                tt("pool", ssmT[:, oc, :], ygT[:, oc, :], sg[:], ALU.mult, [ygT, sg], [ssmT.sub(oc)])
            for oc in range(8):
                pgs = proj(2048 + oc * 128)
                pgcg = proj(3072 + oc * 128)
                pys = bank_rr.next()
                for kc in range(4):
                    mm(pys[:], w_sbr[:, kc, oc * 128:(oc + 1) * 128], ssmT[:, kc, :], kc == 0, kc == 3, [w_sbr, ssmT.sub(kc)], [pys])
                pyc = bank_rr.next()
                for kc in range(4):
                    mm(pyc[:], w_cbr[:, kc, oc * 128:(oc + 1) * 128], convT[:, kc, :], kc == 0, kc == 3, [w_cbr, convT.sub(kc)], [pyc])
                sgs = sgs_r.next()
                sgc = sgc_r.next()
                ms = sgs
                mc_ = sgc
                act(sgs[:], pgs[:], AF.Sigmoid, [pgs], [sgs])
                act(sgc[:], pgcg[:], AF.Sigmoid, [pgcg], [sgc])
                tt("dve", ms[:], pys[:], sgs[:], ALU.mult, [pys, sgs], [ms])
                tt("dve", mc_[:], pyc[:], sgc[:], ALU.mult, [pyc, sgc], [mc_])
                tt("pool", mgT[:, oc, :], ms[:], mc_[:], ALU.add, [ms, mc_], [mgT.sub(oc)])
            for t_ in range(4):
                xo = xo_r.next()
                r0 = base + t_ * 128
                dma("sp", xo[:], x_d[r0:r0 + 128, :], [], [xo])
                for hh in range(2):
                    po = bank_rr.next()
                    for kc in range(8):
                        mm(po[:], mgT[:, kc, t_ * 128:(t_ + 1) * 128], w_o[:, kc, hh * 512:(hh + 1) * 512], kc == 0, kc == 7, [mgT.sub(kc), w_o], [po])
                    tt("dve", xo[:, hh * 512:(hh + 1) * 512], xo[:, hh * 512:(hh + 1) * 512], po[:], ALU.add, [po, xo], [xo])
                dma("act", x1_s[r0:r0 + 128, :], xo[:], [xo], [x1_s.sub(r0 // 128)])
        barrier()
        A.release(m)

    stage = dbg.get("stage", 99)
    for b in range(NBL):
        m_as = A.mark()
        AS["uT"] = uT = A.tile("uT", [128, 4, L], BF16)
        AS["uTc"] = A.tile("uTc", [128, 4, CTXL], BF16)
        phase_A(b)
        if "u" in dbg and b == 0:
            o = dbg_tensor("dbg_u", [128, 4 * L])
            uf = A.tile("uf", [128, 4 * L // 8], F32)
            for i in range(8):
                n_ = 4 * L // 8
                cp("dve", uf[:], uT[:].rearrange("p a b -> p (a b)")[:, i * n_:(i + 1) * n_], [uT.sub((c_, t_)) for c_ in range(4) for t_ in range(8)], [uf])
                dma("sp", o[:, i * n_:(i + 1) * n_], uf[:], [uf], [])
            barrier()
        if stage < 2:
            break
        phase_S(b)
        if "yg" in dbg and b == 0:
            o = dbg_tensor("dbg_yg", [128, 4 * L])
            uf = A.tile("uf2", [128, 4 * L // 8], F32)
            for i in range(8):
                n_ = 4 * L // 8
                cp("dve", uf[:], uT[:].rearrange("p a b -> p (a b)")[:, i * n_:(i + 1) * n_], [uT.sub((c_, t_)) for c_ in range(4) for t_ in range(8)], [uf])
                dma("sp", o[:, i * n_:(i + 1) * n_], uf[:], [uf], [])
            barrier()
        if stage < 3:
            break
        A.release(m_as)
        phase_B(b)
        if stage < 4:
            break
    if "x1" in dbg:
        o = dbg_tensor("dbg_x1", [L, D])
        xr = A.ring("xdbg", [128, D], F32, 2)
        for i in range(L // 128):
            t_ = xr.next()
            dma("sp", t_[:], x1_s[i * 128:(i + 1) * 128, :], [x1_s.sub(i)], [t_])
            dma("sp", o[i * 128:(i + 1) * 128, :], t_[:], [t_], [])


    SP_ENG = mybir.EngineType.SP
    SIGMAX = 1.0 / (1.0 + math.exp(-1.702 * 7.0))
    AX = mybir.AxisListType

    def make_row(dst, colfn, diag_r):
        for hh in range(2):
            pg = bank_rr.next()
            for q in range(4):
                j = hh * 4 + q
                dg_ = diag_r.next()
                col, cb = colfn(j)
                ts("dve", dg_[:], identf[:], col, None, ALU.mult, None, [identf, cb], [dg_])
                mm(pg[:, q * 128:(q + 1) * 128], onesf[:], dg_[:], True, True, [onesf, dg_], [pg])
            cp("act", dst[:, hh * 512:(hh + 1) * 512], pg[:], [pg], [dst])

    def phase_moe():
        bgT_s = dscr("bgT_s", [NE * 128, 24], F32)
        conv_step(len(conv_jobs))
        m_moe = A.mark()
        GK = [A.tile("GK%d" % k, [128, 64], F32) for k in range(TOPK)]
        DK = [A.tile("DK%d" % k, [128, 64], I32) for k in range(TOPK)]
        GT = A.tile("GT", [128, 64, NE], F32)
        IDX = A.tile("IDX", [128, NBLK], I32)
        IDX3 = A.tile("IDX3", [128, NBLK], I32)
        bd = A.tile("bd", [NE, D], F32)
        dma("sp", bd[:], b_down_d, [], [bd])
        diag_r = A.ring("diag", [128, 128], F32, 2)
        m_r = A.mark()
        zt = A.tile("zt", [128, 4, D], BF16)
        memset("pool", zt[:], 0.0, [zt])
        xbA = xb_s.sub("all")
        zsubs = [xb_s.sub(("z", i)) for i in range(CAP // 512)]
        for i in range(CAP // 512):
            dma("sp", xb_s[i * 512:(i + 1) * 512, :].rearrange("(n p) d -> p n d", p=128), zt[:], [zt], [zsubs[i]])
        wr = A.tile("wr", [128, 8, NE], F32)
        dma("sp", wr[:], w_router_d.rearrange("(kc p) e -> p kc e", p=128), [], [wr])
        brow = A.tile("brow", [128, NE], F32)
        dma("sp", brow[:], bass.AP(tensor=b_router_d.tensor, offset=0, ap=[[0, 128], [1, NE]]), [], [brow])
        tri = A.tile("tri", [128, 128], F32)
        memset("pool", tri[:], 1.0, [tri])
        S.op("pool", lambda e: e.affine_select(out=tri[:], in_=tri[:], pattern=[[1, 128]], compare_op=ALU.is_gt, fill=0.0, base=0, channel_multiplier=-1), [tri], [tri])
        bg = A.tile("bg", [NE, 2 * D], F32)
        dma("sp", bg[:], b_gu_d, [], [bg])
        bgT = A.tile("bgT", [128, NE, 24], F32)
        for fc in range(16):
            pb = bank_rr.next()
            tr(pb[:, 0:NE], bg[0:NE, fc * 128:(fc + 1) * 128], identf[0:NE, 0:NE], [bg, identf], [pb])
            if fc < 8:
                ts("dve", bgT[:, :, fc], pb[:, 0:NE], 1.702, None, ALU.mult, None, [pb], [bgT])
                cp("act", bgT[:, :, 8 + fc], pb[:, 0:NE], [pb], [bgT])
            else:
                ts("dve", bgT[:, :, 8 + fc], pb[:, 0:NE], 1.0, None, ALU.add, None, [pb], [bgT])
        dma("sp", bgT_s[:, :].rearrange("(e p) c -> p e c", p=128), bgT[:], [bgT], [bgT_s])
        gs_row = [A.tile("gs_row%d" % b, [128, D], F32) for b in range(NBL)]
        sh_row = [A.tile("sh_row%d" % b, [128, D], F32) for b in range(NBL)]
        for b in range(NBL):
            make_row(gs_row[b], lambda j, b=b: (gsf[:, j, b:b + 1], gsf), diag_r)
            make_row(sh_row[b], lambda j, b=b: (modT[:, 24 + j, b:b + 1], modT), diag_r)
        LG = A.tile("LG", [128, 64, NE], F32)
        MK = A.tile("MK", [128, 64, NE], F32)
        RK = A.tile("RK", [128, 64, NE], F32)
        M8 = A.tile("M8", [128, 64, 8], F32)
        Macc = A.tile("Macc", [128, NE], F32)
        xt_r = A.ring("xt", [128, D], F32, 4)
        hf_r = A.ring("hf", [128, D], F32, 3)
        hb_r = A.ring("hbt", [128, D], BF16, 4)
        hlo_r = A.ring("hlo", [128, D], BF16, 3)
        hTh_r = A.ring("hTh", [128, 8, 128], BF16, 2)
        hTl_r = A.ring("hTl", [128, 8, 128], BF16, 2)
        wr_hi = A.tile("wr_hi", [128, 8, NE], BF16)
        wr_lo = A.tile("wr_lo", [128, 8, NE], BF16)
        cp("dve", wr_hi[:], wr[:], [wr], [wr_hi])
        tt("dve", wr_lo[:], wr[:], wr_hi[:], ALU.subtract, [wr, wr_hi], [wr_lo])
        ss_r = A.ring("ss", [128, 8], F32, 5)
        E_r = A.ring("E", [128, NE], F32, 2)
        CS = A.tile("CS", [128, 64, NE], F32)
        CS2 = A.tile("CS2", [128, 64, NE], F32)

        def r_tile(i):
            b = i // 32
            st = {}

            def r0():
                st["xt"] = xt = xt_r.next(); st["hf"] = hf_r.next(); st["hbt"] = hbt = hb_r.next(); st["ss"] = ss = ss_r.next()
                dma("sp", xt[:], x1_s[i * 128:(i + 1) * 128, :], [x1_s.sub(i)], [xt])
                memset("pool", ss[:], 0.0, [ss])
                act(hbt[:], xt[:], AF.Square, [xt, ss], [hbt, ss], accum=ss[:, 0:1])
                act(ss[:, 1:2], ss[:, 0:1], AF.Ln, [ss], [ss], scale=1.0 / D, bias=epsc[:, 0:1])
                act(ss[:, 2:3], ss[:, 1:2], AF.Exp, [ss], [ss], scale=-0.5)

            def r0b():
                xt, hf, ss = st["xt"], st["hf"], st["ss"]
                stt(hf[:], xt[:], ss[:, 2:3], gs_row[b][:], ALU.mult, ALU.mult, [xt, ss, gs_row[b]], [hf])
                tt("pool", hf[:], hf[:], sh_row[b][:], ALU.add, [hf, sh_row[b]], [hf])

            def r0c():
                hf, hbt = st["hf"], st["hbt"]
                cp("act", hbt[:], hf[:], [hf], [hbt])
                dma("act", hb_s[i * 128:(i + 1) * 128, :], hbt[:], [hbt], [hb_s.sub(i)])
                st["hlo"] = hlo = hlo_r.next()
                tt("dve", hlo[:], hf[:], hbt[:], ALU.subtract, [hf, hbt], [hlo])
                st["pA"] = pA = bank_rr.next(); st["pB"] = pB = bank_rr.next()
                pAb = pA.h.bitcast(BF16); pBb = pB.h.bitcast(BF16)
                for kc in range(8):
                    tr(pAb[:, kc * 128:(kc + 1) * 128], hbt[:, kc * 128:(kc + 1) * 128], identb[:], [hbt, identb], [pA])
                for kc in range(8):
                    tr(pBb[:, kc * 128:(kc + 1) * 128], hlo[:, kc * 128:(kc + 1) * 128], identb[:], [hlo, identb], [pB])

            def r1():
                st["hTh"] = hTh = hTh_r.next(); st["hTl"] = hTl = hTl_r.next()
                cp("act", hTh[:].rearrange("p a t -> p (a t)"), st["pA"].h.bitcast(BF16)[:, 0:1024], [st["pA"]], [hTh])
                cp("dve", hTl[:].rearrange("p a t -> p (a t)"), st["pB"].h.bitcast(BF16)[:, 0:1024], [st["pB"]], [hTl])
                st["pl"] = pl = bank_rr.next()
                terms = [(hTh, wr_hi), (hTl, wr_hi), (hTh, wr_lo)]
                n_ = 0
                for (hx_, wx_) in terms:
                    for kc in range(8):
                        mm(pl[:, 0:NE], hx_[:, kc, :], wx_[:, kc, :], n_ == 0, n_ == 23, [hx_, wx_], [pl])
                        n_ += 1

            def r2():
                lgi = LG.sub(i); mki = MK.sub(i)
                tt("dve", LG[:, i, :], st["pl"][:, 0:NE], brow[:], ALU.add, [st["pl"], brow], [lgi])
                S.op("dve", lambda e: e.max(out=M8[:, i, :], in_=LG[:, i, :]), [lgi], [M8.sub(i)])
                ts("dve", MK[:, i, :], LG[:, i, :], M8[:, i, 3:4], None, ALU.is_ge, None, [lgi, M8.sub(i)], [mki])
                st["pr"] = pr = bank_rr.next()
                mm(pr[:, 0:NE], tri[:], MK[:, i, :], True, True, [tri, mki], [pr])
                mm(pr[:, NE:2 * NE], onesf[:], MK[:, i, :], True, True, [onesf, mki], [pr])

            def r3():
                cp("act", RK[:, i, :], st["pr"][:, 0:NE], [st["pr"]], [RK.sub(i)])
                cp("act", CS[:, i, :], st["pr"][:, NE:2 * NE], [st["pr"]], [CS.sub(i)])
            return [r0, r0b, r0c, r1, r2, r3]

        rt = [r_tile(i) for i in range(64)]
        for step in range(64 + 5):
            for k_ in range(6):
                ui = step - k_
                if 0 <= ui < 64:
                    rt[ui][k_]()
        allsub = lambda T_: [T_.sub(i) for i in range(64)]
        OH = A.tile("OH", [128, 64, NE], F32)
        PR = A.tile("PR", [128, 64, NE], F32)
        rsum = A.tile("rsum", [128, 64], F32)
        tt("dve", OH[:], LG[:], M8[:, :, 0:1].to_broadcast([128, 64, NE]), ALU.subtract, allsub(LG) + allsub(M8), [OH])
        act(OH[:], OH[:], AF.Exp, [OH], [OH])
        tt("pool", OH[:], OH[:], MK[:], ALU.mult, [OH] + allsub(MK), [OH])
        S.op("dve", lambda e: e.reduce_sum(out=rsum[:], in_=OH[:], axis=AX.X), [OH], [rsum])
        S.op("dve", lambda e: e.reciprocal(out=rsum[:], in_=rsum[:]), [rsum], [rsum])
        tt("dve", GT[:], OH[:], rsum[:].rearrange("p (a o) -> p a o", o=1).to_broadcast([128, 64, NE]), ALU.mult, [OH, rsum], allsub(GT))
        src, dst = CS, CS2
        first = True
        for sh in (1, 2, 4, 8, 16, 32):
            R_ = allsub(src) if first else [src]
            cp("pool", dst[:, 0:sh, :], src[:, 0:sh, :], R_, [dst])
            tt("dve", dst[:, sh:64, :], src[:, sh:64, :], src[:, 0:64 - sh, :], ALU.add, R_, [dst])
            src, dst = dst, src
            first = False
        PFX = src
        tt("dve", RK[:, 1:64, :], RK[:, 1:64, :], PFX[:, 0:63, :], ALU.add, allsub(RK) + [PFX], [RK])
        sm = {k: A.tile("sm" + k, [128, NE], F32) for k in ("nb", "pad", "pe", "ps", "one")}
        smi = A.tile("smi", [128, NE], I32)

        class _C:
            pass
        cntp = _C()
        cntp_ap = PFX[:, 63, :]
        ts("dve", sm["nb"][:], cntp_ap, 1.0 / 128, 0.49609375, ALU.mult, ALU.add, [PFX], [sm["nb"]])
        cp("pool", smi[:], sm["nb"][:], [sm["nb"]], [smi])
        cp("pool", sm["nb"][:], smi[:], [smi], [sm["nb"]])
        ts("dve", sm["pad"][:], sm["nb"][:], 128.0, None, ALU.mult, None, [sm["nb"]], [sm["pad"]])
        memset("pool", sm["one"][:], 1.0, [sm["one"]])
        S.op("dve", lambda e: e.tensor_tensor_scan(out=sm["pe"][:], data0=sm["one"][:], data1=sm["pad"][:], initial=0.0, op0=ALU.mult, op1=ALU.add), [sm["one"], sm["pad"]], [sm["pe"]])
        tt("dve", sm["ps"][:], sm["pe"][:], sm["pad"][:], ALU.subtract, [sm["pe"], sm["pad"]], [sm["ps"]])
        tt("dve", RK[:], RK[:], sm["ps"][:].rearrange("p (o e) -> p o e", o=1).to_broadcast([128, 64, NE]), ALU.add, [RK, sm["ps"]], [RK])
        DKf = A.tile("DKf", [128, 64], F32)
        for k in range(TOPK):
            tt("dve", OH[:], LG[:], M8[:, :, k:k + 1].to_broadcast([128, 64, NE]), ALU.is_equal, allsub(LG) + allsub(M8), [OH])
            tt("pool", PR[:], OH[:], RK[:], ALU.mult, [OH, RK], [PR])
            S.op("dve", lambda e: e.reduce_sum(out=DKf[:], in_=PR[:], axis=AX.X), [PR], [DKf])
            cp("dve", DK[k][:], DKf[:], [DKf], [DK[k]])
            tt("pool", PR[:], OH[:], GT[:], ALU.mult, [OH] + allsub(GT), [PR])
            S.op("dve", lambda e, k=k: e.reduce_sum(out=GK[k][:], in_=PR[:], axis=AX.X), [PR], [GK[k]])
        bs = A.tile("bs", [128, NBLK], F32)
        be = A.tile("be", [128, NBLK], F32)
        chg = A.tile("chg", [128, NBLK], F32)
        pcol = A.tile("pcol", [128, 1], F32)
        S.op("pool", lambda e: e.iota(bs[:], pattern=[[128, NBLK]], base=0, channel_multiplier=0, allow_small_or_imprecise_dtypes=True), [], [bs])
        S.op("pool", lambda e: e.iota(pcol[:], pattern=[[0, 1]], base=0, channel_multiplier=1, allow_small_or_imprecise_dtypes=True), [], [pcol])
        memset("pool", be[:], 0.0, [be])
        for e_ in range(NE):
            stt(be[:], bs[:], sm["pe"][:, e_:e_ + 1], be[:], ALU.is_ge, ALU.add, [bs, sm["pe"], be], [be])
        ts("dve", be[:], be[:], float(NE - 1), None, ALU.min, None, [be], [be])
        memset("pool", chg[:], 0.0, [chg])
        tt("dve", chg[:, 2:NBLK], be[:, 2:NBLK], be[:, 0:NBLK - 2], ALU.is_equal, [be, chg], [chg])
        ts("dve", bs[:], be[:], 128.0, pcol[:, 0:1], ALU.mult, ALU.add, [be, pcol, bs], [bs])
        stt(bs[:], chg[:], 1.0e6, bs[:], ALU.mult, ALU.add, [chg, bs], [bs])
        cp("dve", IDX[:], bs[:], [bs], [IDX])
        memset("pool", chg[:], 0.0, [chg])
        tt("dve", chg[:, 3:NBLK], be[:, 3:NBLK], be[:, 0:NBLK - 3], ALU.is_equal, [be, chg], [chg])
        ts("dve", bs[:], be[:], 128.0, pcol[:, 0:1], ALU.mult, ALU.add, [be, pcol, bs], [bs])
        stt(bs[:], chg[:], 1.0e6, bs[:], ALU.mult, ALU.add, [chg, bs], [bs])
        cp("dve", IDX3[:], bs[:], [bs], [IDX3])
        if "route" in dbg:
            o = dbg_tensor("dbg_route", [128, 64 * NE * 2 + 64 * 4 * 2 + 2 * NBLK])
            dma("sp", o[:, 0:64 * NE], LG[:].rearrange("p a e -> p (a e)"), allsub(LG), [])
            dma("sp", o[:, 64 * NE:2 * 64 * NE], GT[:].rearrange("p a e -> p (a e)"), allsub(GT), [])
            for k in range(TOPK):
                dma("sp", o[:, 2 * 64 * NE + k * 64:2 * 64 * NE + (k + 1) * 64], DKf[:] if False else GK[k][:], [GK[k]], [])
            dkf2 = A.tile("dkf2", [128, 4, 64], F32)
            for k in range(TOPK):
                cp("dve", dkf2[:, k, :], DK[k][:], [DK[k]], [dkf2])
            dma("sp", o[:, 2 * 64 * NE + 256:2 * 64 * NE + 512], dkf2[:].rearrange("p a b -> p (a b)"), [dkf2], [])
            dma("sp", o[0:1, 2 * 64 * NE + 512:2 * 64 * NE + 512 + NBLK], be[0:1, :], [be], [])
            dma("sp", o[0:1, 2 * 64 * NE + 512 + NBLK:2 * 64 * NE + 512 + 2 * NBLK], chg[0:1, :], [chg], [])
        if dbg.get("moe_stop") == "R":
            return
        regs_c = {}

        def creg(e):
            if "c" not in regs_c:
                regs_c["c"] = e.to_reg(CAP - 1)
            return regs_c["c"]
        hs_r = A.ring("hsc", [128, D], BF16, 4)
        for i in range(64):
            ht = hs_r.next()
            dma("sp", ht[:], hb_s[i * 128:(i + 1) * 128, :], [hb_s.sub(i)], [ht])
            for k in range(TOPK):
                S.op("pool", lambda e, ht=ht, k=k, i=i: e.indirect_dma_start(
                    out=xb_s[:, :], out_offset=bass.IndirectOffsetOnAxis(ap=DK[k][:, i:i + 1], axis=0), in_=ht[:], in_offset=None,
                    bounds_check=creg(e), oob_is_err=False), [ht, DK[k], xbA] + zsubs, [], dma=True)
        memset("pool", bar_t[:], 0.0, [xbA])
        barrier()
        A.release(m_r)
        if dbg.get("moe_stop") == "S":
            return
        m_e = A.mark()
        wgu = [A.tile("wgu%d" % s_, [128, 8, 2 * D], BF16) for s_ in range(3)]
        wdn = [A.tile("wdn%d" % s_, [128, 8, D], BF16) for s_ in range(2)]
        bcol = [A.tile("bcol%d" % s_, [128, 24], F32) for s_ in range(3)]
        xb_r = A.ring("xbt", [128, D], BF16, 4)
        hTb_r = A.ring("hTb", [128, 8, 128], BF16, 2)
        gcl_r = A.ring("gcl", [128, 256], F32, 4)
        sig_r = A.ring("sig", [128, 256], F32, 4)
        u1_r = A.ring("u1", [128, 256], F32, 4)
        actT_r = A.ring("actT", [128, 8, 128], BF16, 2)
        yo_r = A.ring("yo", [128, D], F32, 2)
        tbanks = Ring([banks[0], banks[7]])
        gbanks = Ring(banks[1:5])
        dbanks = [banks[5], banks[6]]
        regs = {}

        def breg(e):
            if "w" not in regs:
                regs["w"] = e.to_reg(NE * 128 - 1)
            return regs["w"]
        conv_gu = [wgu_s.sub(e_) for e_ in range(NE)]
        conv_dn = [wdn_s.sub(e_) for e_ in range(NE)]

        def wload_gu(b_):
            s3 = b_ % 3
            S.op("pool", lambda e: e.indirect_dma_start(out=wgu[s3][:].rearrange("p a n -> p (a n)"), out_offset=None, in_=wgu_s[:, :],
                 in_offset=bass.IndirectOffsetOnAxis(ap=IDX3[:, b_:b_ + 1], axis=0), bounds_check=breg(e), oob_is_err=False), [IDX3] + conv_gu, [wgu[s3]], dma=True)
            S.op("pool", lambda e: e.indirect_dma_start(out=bcol[s3][:], out_offset=None, in_=bgT_s[:, :],
                 in_offset=bass.IndirectOffsetOnAxis(ap=IDX3[:, b_:b_ + 1], axis=0), bounds_check=breg(e), oob_is_err=False), [IDX3, bgT_s], [bcol[s3]], dma=True)

        def wload_dn(b_):
            s_ = b_ % 2
            S.op("pool", lambda e: e.indirect_dma_start(out=wdn[s_][:].rearrange("p a n -> p (a n)"), out_offset=None, in_=wdn_s[:, :],
                 in_offset=bass.IndirectOffsetOnAxis(ap=IDX[:, b_:b_ + 1], axis=0), bounds_check=breg(e), oob_is_err=False), [IDX] + conv_dn, [wdn[s_]], dma=True)

        def xload(b_):
            t_ = xb_r.next()
            dma("sp", t_[:], xb_s[b_ * 128:(b_ + 1) * 128, :], [xbA], [t_])
            return t_

        def down(b_, actT):
            s_ = b_ % 2
            for fc in range(8):
                for hh in range(2):
                    mm(dbanks[hh][:], actT[:, fc, :], wdn[s_][:, fc, hh * 512:(hh + 1) * 512], fc == 0, fc == 7, [actT, wdn[s_]], [dbanks[hh]])
            yo = yo_r.next()
            cp("act", yo[:, 0:512], dbanks[0][:], [dbanks[0]], [yo])
            cp("act", yo[:, 512:1024], dbanks[1][:], [dbanks[1]], [yo])
            dma("act", yb_s[b_ * 128:(b_ + 1) * 128, :], yo[:], [yo], [yb_s.sub(b_)])

        nblk = dbg.get("nblk", NBLK)
        ebis = dbg.get("ebis", 9)
        if ebis == 0:
            return
        wload_gu(0); wload_gu(1); wload_gu(2)
        wload_dn(0); wload_dn(1)
        if ebis == 1:
            o = dbg_tensor("dbg_w", [128, 24 + 2048 + 1024])
            wtmp = A.tile("wtmp", [128, 2048 + 1024], F32)
            cp("dve", wtmp[:, 0:2048], wgu[0][:, 3, :], [wgu[0]], [wtmp])
            cp("dve", wtmp[:, 2048:3072], wdn[1][:, 5, :], [wdn[1]], [wtmp])
            dma("sp", o[:, 0:24], bcol[0][:], [bcol[0]], [])
            dma("sp", o[:, 24:24 + 3072], wtmp[:], [wtmp], [])
            return
        def transp(xt_):
            tb = tbanks.next()
            tbb = tb.h.bitcast(BF16)
            for kc in range(8):
                tr(tbb[:, kc * 128:(kc + 1) * 128], xt_[:, kc * 128:(kc + 1) * 128], identb[:], [xt_, identb], [tb])
            hT_ = hTb_r.next()
            cp("act", hT_[:].rearrange("p a t -> p (a t)"), tbb[:, 0:1024], [tb], [hT_])
            return hT_

        xq = {0: xload(0)}
        if nblk > 1:
            xq[1] = xload(1)
        hT_next = transp(xq.pop(0))
        prev = None
        for b_ in range(nblk):
            s_ = b_ % 2
            hT = hT_next
            if b_ + 2 < nblk:
                xq[b_ + 2] = xload(b_ + 2)
            if b_ + 1 < nblk:
                hT_next = transp(xq.pop(b_ + 1))
            if ebis == 2:
                continue
            actT = actT_r.next()
            pend_fin = None
            for pg_ in range(4):
                gb_ = gbanks.next()
                fcs = (2 * pg_, 2 * pg_ + 1, 8 + 2 * pg_, 9 + 2 * pg_)
                for q, fc in enumerate(fcs):
                    for kc in range(8):
                        if b_ == 150 and kc == 7 and q == 3:
                            S.tag_next = "mm b150 pg%d last" % pg_
                        mm(gb_[:, q * 128:(q + 1) * 128], wgu[b_ % 3][:, kc, fc * 128:(fc + 1) * 128], hT[:, kc, :], kc == 0, kc == 7, [wgu[b_ % 3], hT], [gb_])
                if ebis == 3:
                    continue
                gcl = gcl_r.next(); sig = sig_r.next(); u1 = u1_r.next()
                for q in range(2):
                    fc = 2 * pg_ + q
                    sl = slice(q * 128, (q + 1) * 128)
                    if b_ == 150 and q == 0:
                        S.tag_next = "gu1 b150 pg%d" % pg_
                    ts("dve", gcl[:, sl], gb_[:, sl], bcol[b_ % 3][:, 8 + fc:9 + fc], 7.0, ALU.add, ALU.min, [gb_, bcol[b_ % 3]], [gcl])
                    ts("dve", u1[:, sl], gb_[:, 256 + q * 128:256 + (q + 1) * 128], bcol[b_ % 3][:, 16 + fc:17 + fc], -6.0, ALU.add, ALU.max, [gb_, bcol[b_ % 3]], [u1])
                act(sig[:], gcl[:], AF.Sigmoid, [gcl], [sig], scale=1.702)

                def fin(pg_=pg_, gcl=gcl, sig=sig, u1=u1):
                    tt("dve", sig[:], sig[:], gcl[:], ALU.mult, [sig, gcl], [sig])
                    stt(actT[:, 2 * pg_:2 * pg_ + 2, :].rearrange("p a t -> p (a t)"), u1[:], 8.0, sig[:], ALU.min, ALU.mult, [u1, sig], [actT])
                if pend_fin is not None:
                    pend_fin()
                pend_fin = fin
            if pend_fin is not None:
                pend_fin()
                pend_fin = None
            if ebis <= 4 or ebis >= 40:
                continue
            if prev is not None:
                down(*prev)
            prev = (b_, actT)
            if b_ >= 1 and b_ + 2 < nblk:
                wload_gu(b_ + 2)
            if b_ >= 1 and b_ + 1 < nblk:
                wload_dn(b_ + 1)
        if prev is not None:
            down(*prev)
        ybJ = yb_s.sub("join")
        memset("pool", bar_t[:], 0.0, [ybJ])
        S.op("pool", lambda e: e.memset(bar_t[:], 0.0), [yb_s.sub(b_) for b_ in range(nblk)], [ybJ])
        barrier()
        A.release(m_e)
        if dbg.get("moe_stop") == "E":
            return
        g5_row = [A.tile("g5_row%d" % b, [128, D], F32) for b in range(NBL)]
        gfin_row = A.tile("gfin_row", [128, D], F32)
        for b in range(NBL):
            make_row(g5_row[b], lambda j, b=b: (modT[:, 40 + j, b:b + 1], modT), diag_r)
        make_row(gfin_row, lambda j: (gfinT[:, j:j + 1], gfinT), diag_r)
        Y_r = A.ring("Y", [128, D], F32, 12)
        acc_r = A.ring("acc", [128, D], F32, 4)
        x1_r = A.ring("x1t", [128, D], F32, 4)
        gT_r = A.ring("gT", [NE, 128], F32, 3)
        ss_r = A.ring("ssg", [128, 4], F32, 4)
        sq_r = A.ring("sqg", [128, D], BF16, 1)

        def g_tile(i):
            b = i // 32
            st = {}

            def g0():
                st["x1t"] = x1t = x1_r.next()
                dma("sp", x1t[:], x1_s[i * 128:(i + 1) * 128, :], [x1_s.sub(i)], [x1t])
                st["Ys"] = Ys = []
                for k in range(TOPK):
                    Y = Y_r.next()
                    S.op("pool", lambda e, Y=Y, k=k: e.indirect_dma_start(
                        out=Y[:], out_offset=None, in_=yb_s[:, :], in_offset=bass.IndirectOffsetOnAxis(ap=DK[k][:, i:i + 1], axis=0),
                        bounds_check=creg(e), oob_is_err=False), [DK[k], ybJ], [Y], dma=True)
                    Ys.append(Y)
                pt = bank_rr.next()
                tr(pt[0:NE, 0:128], GT[:, i, :], identf[:], [GT.sub(i), identf], [pt])
                st["gT"] = gT = gT_r.next()
                cp("act", gT[:], pt[0:NE, 0:128], [pt], [gT])

            def g1():
                Ys = st["Ys"]; gT = st["gT"]
                st["acc"] = acc = acc_r.next()
                for hh in range(2):
                    pbd = bank_rr.next()
                    mm(pbd[:], gT[:], bd[:, hh * 512:(hh + 1) * 512], True, True, [gT, bd], [pbd])
                    stt(acc[:, hh * 512:(hh + 1) * 512], Ys[0][:, hh * 512:(hh + 1) * 512], GK[0][:, i:i + 1], pbd[:], ALU.mult, ALU.add, [Ys[0], GK[0], pbd], [acc])
                for k in range(1, TOPK):
                    stt(acc[:], Ys[k][:], GK[k][:, i:i + 1], acc[:], ALU.mult, ALU.add, [Ys[k], GK[k], acc], [acc])

            def g2():
                acc = st["acc"]
                tt("dve", acc[:], acc[:], g5_row[b][:], ALU.mult, [acc, g5_row[b]], [acc])
                tt("pool", acc[:], acc[:], st["x1t"][:], ALU.add, [acc, st["x1t"]], [acc])
                st["ss"] = ss = ss_r.next(); sq = sq_r.next()
                memset("pool", ss[:], 0.0, [ss])
                act(sq[:], acc[:], AF.Square, [acc, ss], [sq, ss], accum=ss[:, 0:1])
                act(ss[:, 1:2], ss[:, 0:1], AF.Ln, [ss], [ss], scale=1.0 / D, bias=epsc[:, 0:1])
                act(ss[:, 2:3], ss[:, 1:2], AF.Exp, [ss], [ss], scale=-0.5)

            def g3():
                acc = st["acc"]; ss = st["ss"]
                stt(acc[:], acc[:], ss[:, 2:3], gfin_row[:], ALU.mult, ALU.mult, [acc, ss, gfin_row], [acc])
                dma("act", out_d[i * 128:(i + 1) * 128, :], acc[:], [acc], [])
            return [g0, g1, g2, g3]

        gt_ = [g_tile(i) for i in range(64)]
        for step in range(64 + 3):
            for k_ in range(4):
                ui = step - k_
                if 0 <= ui < 64:
                    gt_[ui][k_]()
        A.release(m_moe)

    if stage >= 5:
        barrier()
        phase_moe()
    else:
        zt_ = A.tile("zt_", [128, D], F32)
        memset("pool", zt_[:], 0.0, [zt_])
        dma("sp", out_d[0:128, :], zt_[:], [zt_], [])

    S.finalize()
    return nc, dbg_out, S.stats, A.peak


def make_in_maps(inputs):
    f = lambda a: np.ascontiguousarray(a, dtype=np.float32)
    shared = dict(
        w_mod=f(inputs["w_mod"][0]), b_mod=f(inputs["b_mod"][0]), g_mix=f(inputs["g_mix"][0]), w_in=f(inputs["w_in"][0]),
        lam_re=f(inputs["lam_re"][0]).reshape(64, 64), lam_im=f(inputs["lam_im"][0]).reshape(64, 64),
        log_dt=f(inputs["log_dt"][0]).reshape(1, 64), b_re=f(inputs["b_re"][0]), b_im=f(inputs["b_im"][0]),
        c_re=f(inputs["c_re"][0]).reshape(8, 8, 16, 64), c_im=f(inputs["c_im"][0]).reshape(8, 8, 16, 64),
        d_skip=f(inputs["d_skip"][0]), w_glu=f(inputs["w_glu"][0]), conv_w=f(inputs["conv_w"][0]), conv_b=f(inputs["conv_b"][0]),
        w_ssm_br=f(inputs["w_ssm_br"][0]), w_conv_br=f(inputs["w_conv_br"][0]), w_o=f(inputs["w_o"][0]), g_ffn=f(inputs["g_ffn"][0]),
        w_router=f(inputs["w_router"][0]), b_router=f(inputs["b_router"][0]).reshape(1, NE), w_gu=f(inputs["w_gu"][0]),
        b_gu=f(inputs["b_gu"][0]), w_down=f(inputs["w_down"][0]), b_down=f(inputs["b_down"][0]), g_final=f(inputs["g_final"]),
    )
    maps = []
    for c in range(NCORES):
        bs = slice(c * NBL, (c + 1) * NBL)
        m = dict(shared)
        m["x"] = f(inputs["x"][bs]).reshape(NTOK, D)
        m["ctx"] = f(inputs["ctx"][bs]).reshape(NBL * CTXL, D)
        m["cond3"] = f(np.concatenate([inputs["c"][bs], inputs["c_ctx"][None, :]], axis=0))
        maps.append(m)
    return maps


def kernel(**inputs):
    nc, _, _, _ = build_program()
    res = run_bass_kernel_spmd(nc, make_in_maps(inputs), core_ids=list(range(NCORES)))
    outs = [np.asarray(r["out"]).reshape(NBL, L, D) for r in res.results]
    return np.concatenate(outs, axis=0).astype(np.float32)
```

```python
import math
import numpy as np
import concourse.bass as bass
import concourse.mybir as mybir
from concourse.bass_utils import run_bass_kernel_spmd

F32 = mybir.dt.float32
BF16 = mybir.dt.bfloat16
I32 = mybir.dt.int32
ALU = mybir.AluOpType
AF = mybir.ActivationFunctionType
POOL_ENG = mybir.EngineType.Pool

NCORES = 8
D = 1024
L = 4096
NBL = 2
NTOK = NBL * L
CTXL = 256
NE = 32
TOPK = 4
NSLOT = NTOK * TOPK
NBLK = NSLOT // 128 + NE
CAP = NBLK * 128
TWO_PI_LO = 6.2831845
SBUF_BASE = 16384
SBUF_LIMIT = SBUF_BASE + 206 * 1024
DT_SIZE = {F32: 4, BF16: 2, I32: 4}


class Buf:
    __slots__ = ("name", "lw", "rd")

    def __init__(self, name):
        self.name = name
        self.lw = None
        self.rd = []


class Tl:
    def __init__(self, h, name):
        self.h = h
        self.b = Buf(name)
        self.name = name
        self._sub = {}

    def __getitem__(self, k):
        return self.h[k]

    def sub(self, key):
        s = self._sub.get(key)
        if s is None:
            s = self._sub[key] = Buf("%s/%s" % (self.name, key))
        return s


def _bufs(xs):
    out = []
    for x in xs:
        if x is None:
            continue
        out.append(x.b if isinstance(x, Tl) else x)
    return out


class Sched:
    NDMA = 32

    def __init__(self, nc):
        self.nc = nc
        self.ops = []
        self.engs = {"pe": nc.tensor, "act": nc.scalar, "dve": nc.vector, "pool": nc.gpsimd, "sp": nc.sync}
        self.sems = {k: nc.alloc_semaphore("s_" + k) for k in ("pe", "act", "dve", "pool")}
        self.dsems = [nc.alloc_semaphore("d%d" % i) for i in range(self.NDMA)]

    def op(self, eng, fn, reads=(), writes=(), dma=False):
        self.ops.append((eng, fn, _bufs(reads), _bufs(writes), dma, False))
        if getattr(self, "tag_next", None):
            self.tags = getattr(self, "tags", {})
            self.tags[len(self.ops) - 1] = self.tag_next
            self.tag_next = None

    def barrier(self, fn):
        self.ops.append(("pool", fn, [], [], False, True))

    def finalize(self):
        ops = self.ops
        n = len(ops)
        deps = [None] * n
        needed = [False] * n
        dslot_last = [None] * self.NDMA
        dcount = [0] * self.NDMA
        dinfo = {}
        dk = 0
        last_on = {}
        last_bar = None
        for i, (eng, fn, reads, writes, dma, isbar) in enumerate(ops):
            d = set()
            if isbar:
                d.update(last_on.values())
                d.update(x for x in dslot_last if x is not None)
                last_bar = i
            else:
                if last_bar is not None:
                    d.add(last_bar)
                for b in reads:
                    if b.lw is not None:
                        d.add(b.lw)
                for b in writes:
                    if b.lw is not None:
                        d.add(b.lw)
                    if isinstance(b.rd, dict):
                        for v_ in b.rd.values():
                            if isinstance(v_, list):
                                d.update(v_)
                            else:
                                d.add(v_)
                rkey = "dma" if dma else eng
                for b in reads:
                    if not isinstance(b.rd, dict):
                        b.rd = {}
                    if dma:
                        b.rd.setdefault("dma", []).append(i)
                    else:
                        b.rd[rkey] = i
                for b in writes:
                    b.lw = i
                    b.rd = {}
            if dma:
                k = dk % self.NDMA
                dk += 1
                if dslot_last[k] is not None:
                    d.add(dslot_last[k])
                dslot_last[k] = i
                dcount[k] += 1
                dinfo[i] = (k, dcount[k] * 16)
            else:
                last_on[eng] = i
            d.discard(i)
            if eng == "pe" and not dma:
                d = {j for j in d if not (ops[j][0] == "pe" and not ops[j][4])}
            deps[i] = d
            for j in d:
                needed[j] = True
        cnt = {k: 0 for k in self.sems}
        comp = [None] * n
        for i, (eng, fn, reads, writes, dma, isbar) in enumerate(ops):
            if dma:
                k, v = dinfo[i]
                comp[i] = (("d", k), v)
            elif needed[i]:
                cnt[eng] += 1
                comp[i] = ((eng,), cnt[eng])
        known = {e: {} for e in self.engs}
        nwait = 0
        for i, (eng, fn, reads, writes, dma, isbar) in enumerate(ops):
            e = self.engs[eng]
            kn = known[eng]
            need = {}
            for j in deps[i]:
                key, v = comp[j]
                if kn.get(key, 0) < v:
                    need[key] = max(need.get(key, 0), v)
            if i in getattr(self, "tags", {}):
                print("TAG", self.tags[i], "op", i, eng, "needs", need, "deps", sorted((j, ops[j][0], comp[j]) for j in deps[i]))
            for key, v in need.items():
                sem = self.dsems[key[1]] if key[0] == "d" else self.sems[key[0]]
                e.wait_ge(sem, v)
                kn[key] = v
                nwait += 1
            inst = fn(e)
            if dma:
                k, v = dinfo[i]
                inst.then_inc(self.dsems[k], 16)
            elif needed[i]:
                inst.then_inc(self.sems[eng], 1)
        sp = self.engs["sp"]
        for k in range(self.NDMA):
            if dcount[k]:
                sp.wait_ge(self.dsems[k], dcount[k] * 16)
        for k, v in cnt.items():
            if v:
                sp.wait_ge(self.sems[k], v)
        self.stats = dict(ops=n, waits=nwait, cnt=cnt)


class Arena:
    def __init__(self, nc):
        self.nc = nc
        self.top = SBUF_BASE
        self.n = 0
        self.peak = 0

    def tile(self, name, shape, dtype):
        per = 1
        for s in shape[1:]:
            per *= s
        nbytes = per * DT_SIZE[dtype]
        off = (self.top + 63) // 64 * 64
        assert off + nbytes <= SBUF_LIMIT, ("SBUF overflow", name, off, nbytes)
        self.top = off + nbytes
        self.peak = max(self.peak, self.top)
        self.n += 1
        h = self.nc.alloc_sbuf_tensor_at("%s_%d" % (name, self.n), list(shape), dtype, offset=off)
        return Tl(h, name)

    def ring(self, name, shape, dtype, n):
        return Ring([self.tile("%s%d" % (name, i), shape, dtype) for i in range(n)])

    def mark(self):
        return self.top

    def release(self, m):
        self.top = m


class Ring:
    def __init__(self, tiles):
        self.t = tiles
        self.i = 0

    def next(self):
        t = self.t[self.i % len(self.t)]
        self.i += 1
        return t


def bc_ap(ap, shape):
    return ap.to_broadcast(list(shape))


def build_program(dbg=None):
    nc = bass.Bass("TRN2", target_bir_lowering=False)
    S = Sched(nc)
    A = Arena(nc)
    dbg = dbg or {}

    def din(name, shape, dt=F32):
        return nc.dram_tensor(name, list(shape), dt, kind="ExternalInput").ap()

    x_d = din("x", [NTOK, D])
    cond_d = din("cond3", [3, D])
    ctx_d = din("ctx", [NBL * CTXL, D])
    w_mod_d = din("w_mod", [D, 6 * D])
    b_mod_d = din("b_mod", [6 * D])
    g_mix_d = din("g_mix", [D])
    w_in_d = din("w_in", [D, 4096])
    lam_re_d = din("lam_re", [64, 64])
    lam_im_d = din("lam_im", [64, 64])
    log_dt_d = din("log_dt", [1, 64])
    b_re_d = din("b_re", [2, 32, 64, 16])
    b_im_d = din("b_im", [2, 32, 64, 16])
    c_re_d = din("c_re", [8, 8, 16, 64])
    c_im_d = din("c_im", [8, 8, 16, 64])
    d_skip_d = din("d_skip", [512])
    w_glu_d = din("w_glu", [512, 512])
    conv_w_d = din("conv_w", [3, 512])
    conv_b_d = din("conv_b", [512])
    w_ssm_br_d = din("w_ssm_br", [512, D])
    w_conv_br_d = din("w_conv_br", [512, D])
    w_o_d = din("w_o", [D, D])
    g_ffn_d = din("g_ffn", [D])
    if dbg.get("stage", 99) < 5:
        din_moe = lambda *a, **k: None
    else:
        din_moe = din
    w_router_d = din_moe("w_router", [D, NE])
    b_router_d = din_moe("b_router", [1, NE])
    w_gu_d = din_moe("w_gu", [NE, D, 2 * D])
    b_gu_d = din_moe("b_gu", [NE, 2 * D])
    w_down_d = din_moe("w_down", [NE, D, D])
    b_down_d = din_moe("b_down", [NE, D])
    g_final_d = din("g_final", [D])
    out_d = nc.dram_tensor("out", [NTOK, D], F32, kind="ExternalOutput").ap()

    def dscr(name, shape, dt):
        return Tl(nc.dram_tensor(name, list(shape), dt, kind="Internal").ap(), name)

    x1_s = dscr("x1_s", [NTOK, D], F32)
    hb_s = dscr("hb_s", [NTOK, D], BF16)
    xb_s = dscr("xb_s", [CAP, D], BF16)
    yb_s = dscr("yb_s", [CAP, D], F32)

    wgu_s = dscr("wgu_s", [NE * 128, 8 * 2 * D], BF16)
    wdn_s = dscr("wdn_s", [NE * 128, 8 * D], BF16)
    conv_jobs = [(k_, e_) for e_ in range(NE) for k_ in (0, 1)] if dbg.get("stage", 99) >= 5 else []
    conv_state = [0]

    def conv_step(n):
        for _ in range(n):
            if conv_state[0] >= len(conv_jobs):
                return
            k_, e_ = conv_jobs[conv_state[0]]
            conv_state[0] += 1
            if k_ == 0:
                S.op("pool", lambda e, e_=e_: e.dma_start(out=wgu_s[e_ * 128:(e_ + 1) * 128, :].rearrange("p (kc n) -> p kc n", kc=8),
                                                        in_=w_gu_d[e_].rearrange("(kc p) n -> p kc n", p=128)), [], [wgu_s.sub(e_)], dma=True)
            else:
                S.op("pool", lambda e, e_=e_: e.dma_start(out=wdn_s[e_ * 128:(e_ + 1) * 128, :].rearrange("p (kc n) -> p kc n", kc=8),
                                                        in_=w_down_d[e_].rearrange("(kc p) n -> p kc n", p=128)), [], [wdn_s.sub(e_)], dma=True)

    dbg_out = {}

    def dbg_tensor(name, shape):
        t = nc.dram_tensor(name, list(shape), F32, kind="ExternalOutput").ap()
        dbg_out[name] = t
        return t

    banks = []
    for i in range(8):
        banks.append(Tl(nc.alloc_psum_tensor("bank%d" % i, [128, 512], F32), "bank%d" % i))
    bank_rr = Ring(banks)

    def dma(eng, out, in_, R, W, **kw):
        S.op(eng, lambda e: e.dma_start(out=out, in_=in_, **kw), R, W, dma=True)

    def tt(eng, out, in0, in1, op, R, W):
        S.op(eng, lambda e: e.tensor_tensor(out=out, in0=in0, in1=in1, op=op), R, W)

    def ts(eng, out, in0, s1, s2, op0, op1, R, W):
        if s2 is None:
            S.op(eng, lambda e: e.tensor_scalar(out=out, in0=in0, scalar1=s1, scalar2=None, op0=op0), R, W)
        else:
            S.op(eng, lambda e: e.tensor_scalar(out=out, in0=in0, scalar1=s1, scalar2=s2, op0=op0, op1=op1), R, W)

    def stt(out, in0, scalar, in1, op0, op1, R, W):
        S.op("dve", lambda e: e.scalar_tensor_tensor(out=out, in0=in0, scalar=scalar, in1=in1, op0=op0, op1=op1), R, W)

    def act(out, in_, func, R, W, scale=1.0, bias=None, accum=None):
        def f(e):
            kw = {}
            if bias is not None:
                kw["bias"] = bias
            if accum is not None:
                kw["accum_out"] = accum
            return e.activation(out=out, in_=in_, func=func, scale=scale, **kw)
        S.op("act", f, R, W)

    def cp(eng, out, in_, R, W):
        if eng == "act":
            S.op("act", lambda e: e.copy(out=out, in_=in_), R, W)
        else:
            S.op(eng, lambda e: e.tensor_copy(out=out, in_=in_), R, W)

    def mm(out, lhsT, rhs, start, stop, R, W):
        S.op("pe", lambda e: e.matmul(out, lhsT=lhsT, rhs=rhs, start=start, stop=stop), R, W)

    def tr(out, in_, ident, R, W):
        S.op("pe", lambda e: e.transpose(out=out, in_=in_, identity=ident), R, W)

    def memset(eng, ap, val, W):
        S.op(eng, lambda e: e.memset(ap, val), [], W)

    def barrier():
        S.barrier(lambda e: e.memset(bar_t[:], 0.0))

    bar_t = A.tile("bar", [128, 1], F32)
    identf = A.tile("identf", [128, 128], F32)
    identb = A.tile("identb", [128, 128], BF16)
    onesf = A.tile("onesf", [128, 128], F32)
    pswap = A.tile("pswap", [128, 128], F32)
    sgn = A.tile("sgn", [128, 1], F32)
    nsgn = A.tile("nsgn", [128, 1], F32)
    neg1 = A.tile("neg1", [128, 1], F32)
    epsc = A.tile("epsc", [128, 1], F32)
    magc = A.tile("magc", [128, 4], F32)
    rowmask = A.tile("rowmask", [128, 8], F32)
    modT = A.tile("modT", [128, 48, 3], F32)
    gsm = A.tile("gsm", [128, 8, 3], F32)
    gsf = A.tile("gsf", [128, 8, 3], F32)
    gmT = A.tile("gmT", [128, 8], F32)
    gfT = A.tile("gfT", [128, 8], F32)
    gfinT = A.tile("gfinT", [128, 8], F32)
    dskT = A.tile("dskT", [128, 4], F32)
    cwT = A.tile("cwT", [128, 3, 4], F32)
    cbT = A.tile("cbT", [128, 4], F32)
    rcol = A.tile("rcol", [128, 64], F32)
    fcol = A.tile("fcol", [128, 64], F32)
    BB1T = A.tile("BB1T", [128, 8, 128], BF16)
    BB2T = A.tile("BB2T", [128, 8, 128], BF16)
    W1d = A.tile("W1d", [128, 8, 128], BF16)
    W2d = A.tile("W2d", [128, 8, 128], BF16)

    memset("pool", identf[:], 0.0, [identf])
    S.op("pool", lambda e: e.affine_select(out=identf[:], in_=identf[:], pattern=[[-1, 128]], compare_op=ALU.not_equal, fill=1.0, base=0, channel_multiplier=1), [identf], [identf])
    cp("dve", identb[:], identf[:], [identf], [identb])
    memset("pool", onesf[:], 1.0, [onesf])
    memset("pool", pswap[:], 0.0, [pswap])
    S.op("pool", lambda e: e.affine_select(out=pswap[:], in_=pswap[:], pattern=[[1, 128]], compare_op=ALU.not_equal, fill=1.0, base=-64, channel_multiplier=-1), [pswap], [pswap])
    S.op("pool", lambda e: e.affine_select(out=pswap[:], in_=pswap[:], pattern=[[1, 128]], compare_op=ALU.not_equal, fill=1.0, base=64, channel_multiplier=-1), [pswap], [pswap])
    memset("pool", sgn[:], 1.0, [sgn])
    memset("pool", sgn[0:64, :], -1.0, [sgn])
    memset("pool", nsgn[:], -1.0, [nsgn])
    memset("pool", nsgn[0:64, :], 1.0, [nsgn])
    memset("pool", neg1[:], -1.0, [neg1])
    memset("pool", epsc[:], 1e-6, [epsc])
    memset("pool", magc[:, 0:1], 12582912.0, [magc])
    memset("pool", magc[:, 1:2], -12582912.0, [magc])
    memset("pool", magc[:, 2:3], 0.25, [magc])
    memset("pool", magc[:, 3:4], 0.0, [magc])
    memset("pool", rowmask[:], 1.0, [rowmask])
    S.op("pool", lambda e: e.affine_select(out=rowmask[:], in_=rowmask[:], pattern=[[-16, 8]], compare_op=ALU.is_ge, fill=0.0, base=0, channel_multiplier=1), [rowmask], [rowmask])
    S.op("pool", lambda e: e.affine_select(out=rowmask[:], in_=rowmask[:], pattern=[[16, 8]], compare_op=ALU.is_ge, fill=0.0, base=15, channel_multiplier=-1), [rowmask], [rowmask])

    def load_cols(tile_, ap_slice, src_ap):
        dma("sp", ap_slice, src_ap.rearrange("(c p) -> p c", p=128), [], [tile_], allow_slow_non_contiguous=True)

    load_cols(gmT, gmT[:], g_mix_d)
    load_cols(gfT, gfT[:], g_ffn_d)
    load_cols(gfinT, gfinT[:], g_final_d)
    load_cols(dskT, dskT[:], d_skip_d)
    load_cols(cbT, cbT[:], conv_b_d)
    for k in range(3):
        load_cols(cwT, cwT[:, k, :], conv_w_d[k])

    m_setup = A.mark()
    condT = A.tile("condT", [128, 8, 3], F32)
    scT = A.tile("scT", [128, 8, 3], F32)
    bmT = A.tile("bmT", [128, 48], F32)
    modacc = A.tile("modacc", [128, 48, 3], F32)
    wm = A.ring("wm", [128, 6144], F32, 2)
    for b_ in range(3):
        dma("sp", condT[:, :, b_], cond_d[b_].rearrange("(kc p) -> p kc", p=128), [], [condT], allow_slow_non_contiguous=True)
    load_cols(bmT, bmT[:], b_mod_d)
    act(scT[:], condT[:], AF.Silu, [condT], [scT])
    for kc in range(8):
        w = wm.next()
        dma("sp", w[:], w_mod_d[kc * 128:(kc + 1) * 128, :], [], [w])
        pm = bank_rr.next()
        for jc in range(48):
            mm(pm[:, jc * 3:(jc + 1) * 3], w[:, jc * 128:(jc + 1) * 128], scT[:, kc, :], True, True, [w, scT], [pm])
        if kc == 0:
            cp("dve", modacc[:].rearrange("p a b -> p (a b)"), pm[:, 0:144], [pm], [modacc])
        else:
            tt("dve", modacc[:].rearrange("p a b -> p (a b)"), modacc[:].rearrange("p a b -> p (a b)"), pm[:, 0:144], ALU.add, [pm, modacc], [modacc])
    tt("dve", modT[:], modacc[:], bmT[:].rearrange("p (a o) -> p a o", o=1).to_broadcast([128, 48, 3]), ALU.add, [modacc, bmT], [modT])
    ts("dve", gsm[:], modT[:, 8:16, :], 1.0, None, ALU.add, None, [modT], [gsm])
    tt("dve", gsm[:], gsm[:], gmT[:].rearrange("p (a o) -> p a o", o=1).to_broadcast([128, 8, 3]), ALU.mult, [gsm, gmT], [gsm])
    ts("dve", gsf[:], modT[:, 32:40, :], 1.0, None, ALU.add, None, [modT], [gsf])
    tt("dve", gsf[:], gsf[:], gfT[:].rearrange("p (a o) -> p a o", o=1).to_broadcast([128, 8, 3]), ALU.mult, [gsf, gfT], [gsf])

    if "mod" in dbg:
        o = dbg_tensor("dbg_mod", [128, 48 * 3])
        dma("sp", o, modT[:].rearrange("p a b -> p (a b)"), [modT], [])

    lamn = A.tile("lamn", [64, 2, 128], F32)
    dma("sp", lamn[:, 0, 0:64], lam_re_d, [], [lamn])
    dma("sp", lamn[:, 0, 64:128], lam_re_d, [], [lamn])
    dma("sp", lamn[:, 1, 0:64], lam_im_d, [], [lamn])
    dma("sp", lamn[:, 1, 64:128], lam_im_d, [], [lamn])
    lreT = A.tile("lreT", [128, 64], F32)
    limT = A.tile("limT", [128, 64], F32)
    pl = bank_rr.next()
    tr(pl[:, 0:64], lamn[:, 0, :], identf[0:64, 0:64], [lamn, identf], [pl])
    tr(pl[:, 64:128], lamn[:, 1, :], identf[0:64, 0:64], [lamn, identf], [pl])
    cp("dve", lreT[:], pl[:, 0:64], [pl], [lreT])
    cp("dve", limT[:], pl[:, 64:128], [pl], [limT])
    dtb = A.tile("dtb", [128, 64], F32)
    dma("sp", dtb[:], bass.AP(tensor=log_dt_d.tensor, offset=0, ap=[[0, 128], [1, 64]]), [], [dtb])
    act(dtb[:], dtb[:], AF.Exp, [dtb], [dtb])
    s5 = {k: A.tile("s5" + k, [128, 64], F32) for k in "ang lrd t ks fs sn t2 fc cs are aim den am1 qre qim tmp tmp2 sq nq".split()}
    s5i = A.tile("s5i", [128, 64], I32)
    tt("dve", s5["ang"][:], limT[:], dtb[:], ALU.mult, [limT, dtb], [s5["ang"]])
    tt("dve", s5["lrd"][:], lreT[:], dtb[:], ALU.mult, [lreT, dtb], [s5["lrd"]])
    act(rcol[:], s5["lrd"][:], AF.Exp, [s5["lrd"]], [rcol])
    ts("dve", fcol[:], s5["ang"][:], 1.0 / (2 * math.pi), None, ALU.mult, None, [s5["ang"]], [fcol])
    cp("pool", s5i[:], fcol[:], [fcol], [s5i])
    tt("pool", s5["fs"][:], fcol[:], s5i[:], ALU.subtract, [fcol, s5i], [s5["fs"]])
    act(s5["sn"][:], s5["fs"][:], AF.Sin, [s5["fs"]], [s5["sn"]], scale=TWO_PI_LO)
    ts("pool", s5["t2"][:], fcol[:], 0.25, None, ALU.add, None, [fcol], [s5["t2"]])
    cp("pool", s5i[:], s5["t2"][:], [s5["t2"], s5["fs"]], [s5i])
    tt("pool", s5["fc"][:], s5["t2"][:], s5i[:], ALU.subtract, [s5["t2"], s5i], [s5["fc"]])
    act(s5["cs"][:], s5["fc"][:], AF.Sin, [s5["fc"]], [s5["cs"]], scale=TWO_PI_LO)
    tt("dve", s5["are"][:], rcol[:], s5["cs"][:], ALU.mult, [rcol, s5["cs"]], [s5["are"]])
    tt("dve", s5["aim"][:], rcol[:], s5["sn"][:], ALU.mult, [rcol, s5["sn"]], [s5["aim"]])
    tt("dve", s5["den"][:], lreT[:], lreT[:], ALU.mult, [lreT], [s5["den"]])
    tt("dve", s5["tmp"][:], limT[:], limT[:], ALU.mult, [limT], [s5["tmp"]])
    tt("dve", s5["den"][:], s5["den"][:], s5["tmp"][:], ALU.add, [s5["den"], s5["tmp"]], [s5["den"]])
    S.op("dve", lambda e: e.reciprocal(out=s5["den"][:], in_=s5["den"][:]), [s5["den"]], [s5["den"]])
    ts("dve", s5["am1"][:], s5["are"][:], -1.0, None, ALU.add, None, [s5["are"]], [s5["am1"]])
    tt("dve", s5["tmp"][:], s5["am1"][:], lreT[:], ALU.mult, [s5["am1"], lreT], [s5["tmp"]])
    tt("dve", s5["tmp2"][:], s5["aim"][:], limT[:], ALU.mult, [s5["aim"], limT], [s5["tmp2"]])
    tt("dve", s5["tmp"][:], s5["tmp"][:], s5["tmp2"][:], ALU.add, [s5["tmp"], s5["tmp2"]], [s5["tmp"]])
    tt("dve", s5["qre"][:], s5["tmp"][:], s5["den"][:], ALU.mult, [s5["tmp"], s5["den"]], [s5["qre"]])
    tt("dve", s5["tmp"][:], s5["aim"][:], lreT[:], ALU.mult, [s5["aim"], lreT], [s5["tmp"]])
    tt("dve", s5["tmp2"][:], s5["am1"][:], limT[:], ALU.mult, [s5["am1"], limT], [s5["tmp2"]])
    tt("dve", s5["tmp"][:], s5["tmp"][:], s5["tmp2"][:], ALU.subtract, [s5["tmp"], s5["tmp2"]], [s5["tmp"]])
    tt("dve", s5["qim"][:], s5["tmp"][:], s5["den"][:], ALU.mult, [s5["tmp"], s5["den"]], [s5["qim"]])
    ts("dve", s5["sq"][:], s5["qim"][:], sgn[:, 0:1], None, ALU.mult, None, [s5["qim"], sgn], [s5["sq"]])
    ts("dve", s5["nq"][:], s5["qre"][:], nsgn[:, 0:1], None, ALU.mult, None, [s5["qre"], nsgn], [s5["nq"]])
    Ba = A.tile("Ba", [128, 64, 16], F32)
    Bb = A.tile("Bb", [128, 64, 16], F32)
    bsrc_re = b_re_d.rearrange("d g p c -> p (d g) c")
    bsrc_im = b_im_d.rearrange("d g p c -> p (d g) c")
    dma("sp", Ba[0:64], bsrc_re, [], [Ba])
    dma("sp", Ba[64:128], bsrc_im, [], [Ba])
    dma("sp", Bb[0:64], bsrc_im, [], [Bb])
    dma("sp", Bb[64:128], bsrc_re, [], [Bb])
    BB1 = A.tile("BB1", [128, 64, 16], F32)
    BB2 = A.tile("BB2", [128, 64, 16], F32)
    btmp = A.tile("btmp", [128, 64, 16], F32)

    def qb(name):
        return s5[name][:].rearrange("p (a o) -> p a o", o=1).to_broadcast([128, 64, 16])
    tt("dve", BB1[:], Ba[:], qb("qre"), ALU.mult, [Ba, s5["qre"]], [BB1])
    tt("dve", btmp[:], Bb[:], qb("sq"), ALU.mult, [Bb, s5["sq"]], [btmp])
    tt("dve", BB1[:], BB1[:], btmp[:], ALU.add, [BB1, btmp], [BB1])
    tt("dve", BB2[:], Bb[:], qb("nq"), ALU.mult, [Bb, s5["nq"]], [BB2])
    tt("dve", btmp[:], Ba[:], qb("qim"), ALU.mult, [Ba, s5["qim"], BB1], [btmp])
    tt("dve", BB2[:], BB2[:], btmp[:], ALU.add, [BB2, btmp], [BB2])
    for dcc in range(8):
        for (src, dst) in ((BB1, BB1T), (BB2, BB2T)):
            pb = bank_rr.next()
            tr(pb[:, 0:128], src[:, dcc * 8:(dcc + 1) * 8, :].rearrange("p a c -> p (a c)"), identf[:], [src, identf], [pb])
            cp("act", dst[:, dcc, :], pb[:, 0:128], [pb], [dst])
    Cn1 = A.tile("Cn1", [128, 8, 128], F32)
    Cn2 = A.tile("Cn2", [128, 8, 128], F32)
    csrc_re = c_re_d.rearrange("a gl c p -> (gl c) a p")
    csrc_im = c_im_d.rearrange("a gl c p -> (gl c) a p")
    dma("sp", Cn1[:, :, 0:64], csrc_re, [], [Cn1])
    dma("sp", Cn1[:, :, 64:128], csrc_im, [], [Cn1])
    dma("sp", Cn2[:, :, 0:64], csrc_im, [], [Cn2])
    dma("sp", Cn2[:, :, 64:128], csrc_re, [], [Cn2])
    for dcc in range(8):
        pb = bank_rr.next()
        tr(pb[:, 0:128], Cn1[:, dcc, :], identf[:], [Cn1, identf], [pb])
        ts("dve", W1d[:, dcc, :], pb[:, 0:128], nsgn[:, 0:1], None, ALU.mult, None, [pb, nsgn], [W1d])
        pb = bank_rr.next()
        tr(pb[:, 0:128], Cn2[:, dcc, :], identf[:], [Cn2, identf], [pb])
        ts("dve", W2d[:, dcc, :], pb[:, 0:128], neg1[:, 0:1], None, ALU.mult, None, [pb, neg1], [W2d])

    barrier()
    A.release(m_setup)

    yg_s = dscr("yg_s", [128, 4, L], BF16)
    AS = {}

    def norm_tiles(arena_tiles, row_ap_fn, ntile, bidx, hxT, gs_t, sh_lo):
        xt_r, xn_r, sq_r, ss_r = arena_tiles
        stt_ = [dict() for _ in range(ntile)]

        def nA(t_):
            st = stt_[t_]
            st["xt"] = xt = xt_r.next()
            dma("sp", xt[:], row_ap_fn(t_), [], [xt])
            st["ss"] = ss = ss_r.next()
            st["xn"] = xn = xn_r.next()
            memset("pool", ss[:], 0.0, [ss])
            act(xn[:], xt[:], AF.Square, [xt, ss], [xn, ss], accum=ss[:, 0:1])
            act(ss[:, 1:2], ss[:, 0:1], AF.Ln, [ss], [ss], scale=1.0 / D, bias=epsc[:, 0:1])
            act(ss[:, 2:3], ss[:, 1:2], AF.Exp, [ss], [ss], scale=-0.5)

        def nB(t_):
            st = stt_[t_]
            xt, xn, ss = st["xt"], st["xn"], st["ss"]
            ts("dve", xn[:], xt[:], ss[:, 2:3], None, ALU.mult, None, [xt, ss], [xn])
            st["pT"] = pT = bank_rr.next()
            pTb = pT.h.bitcast(BF16)
            for kc in range(8):
                tr(pTb[:, kc * 128:(kc + 1) * 128], xn[:, kc * 128:(kc + 1) * 128], identb[:], [xn, identb], [pT])

        def nC(t_):
            pT = stt_[t_]["pT"]
            pTb = pT.h.bitcast(BF16)
            for kc in range(8):
                dst = hxT[:, kc, t_ * 128:(t_ + 1) * 128]
                src = pTb[:, kc * 128:(kc + 1) * 128]
                if kc % 2 == 0:
                    act(dst, src, AF.Identity, [pT, gs_t, modT], [hxT], scale=gs_t[:, kc, bidx:bidx + 1], bias=modT[:, sh_lo + kc, bidx:bidx + 1])
                else:
                    ts("dve", dst, src, gs_t[:, kc, bidx:bidx + 1], modT[:, sh_lo + kc, bidx:bidx + 1], ALU.mult, ALU.add, [pT, gs_t, modT], [hxT])
        for step in range(ntile + 2):
            if step < ntile:
                nA(step)
            if 0 <= step - 1 < ntile:
                nB(step - 1)
            if 0 <= step - 2 < ntile:
                nC(step - 2)

    def phase_A(b):
        uT, uTc = AS["uT"], AS["uTc"]
        m = A.mark()
        w_in_u = A.tile("w_in_u", [128, 8, 512], BF16)
        dma("pool", w_in_u[:], w_in_d[:, 0:512].rearrange("(kc p) n -> p kc n", p=128), [], [w_in_u])
        tiles = (A.ring("xt", [128, D], F32, 3), A.ring("xn", [128, D], BF16, 3), None, A.ring("ss", [128, 4], F32, 4))
        hx_r = A.ring("hxT", [128, 8, 512], BF16, 2)
        groups = [("ctx", None)] + [("x", tg) for tg in range(8)]
        for kind, tg in groups:
            conv_step(2)
            hxT = hx_r.next()
            if kind == "ctx":
                norm_tiles(tiles, lambda t_: ctx_d[b * CTXL + t_ * 128: b * CTXL + (t_ + 1) * 128, :], 2, 2, hxT, gsm, 0)
                ntok = 256
            else:
                base = b * L + tg * 512
                norm_tiles(tiles, lambda t_: x_d[base + t_ * 128: base + (t_ + 1) * 128, :], 4, b, hxT, gsm, 0)
                ntok = 512
            for cc in range(4):
                pu = bank_rr.next()
                for kc in range(8):
                    mm(pu[:, 0:ntok], w_in_u[:, kc, cc * 128:(cc + 1) * 128], hxT[:, kc, 0:ntok], kc == 0, kc == 7, [w_in_u, hxT], [pu])
                if kind == "ctx":
                    cp("act" if cc % 2 else "dve", uTc[:, cc, :], pu[:, 0:256], [pu], [uTc.sub(cc)])
                else:
                    cp("act" if cc % 2 else "dve", uT[:, cc, tg * 512:(tg + 1) * 512], pu[:, 0:512], [pu], [uT.sub((cc, tg))])
        barrier()
        A.release(m)

    def phase_S(b):
        uT, uTc = AS["uT"], AS["uTc"]
        m = A.mark()
        jrow = A.tile("jrow", [128, 2, 512], F32)
        S.op("pool", lambda e: e.iota(jrow[:, 0, :], pattern=[[1, 512]], base=1, channel_multiplier=0, allow_small_or_imprecise_dtypes=True), [], [jrow])
        S.op("pool", lambda e: e.iota(jrow[:, 1, :], pattern=[[-1, 512]], base=512, channel_multiplier=0, allow_small_or_imprecise_dtypes=True), [], [jrow])
        COS = A.tile("COS", [128, 16, 512], F32)
        SIN = A.tile("SIN", [128, 16, 512], F32)
        ccol = A.tile("ccol", [128, 16, 2], F32)
        scol = A.tile("scol", [128, 16, 2], F32)
        X1w = A.tile("X1w", [128, 16, 128], BF16)
        X2w = A.tile("X2w", [128, 16, 128], BF16)
        W1p = A.tile("W1p", [128, 16, 128], BF16)
        W2p = A.tile("W2p", [128, 16, 128], BF16)
        tg_t = A.ring("tgt", [128, 512], F32, 3)
        tg_i = A.ring("tgi", [128, 512], F32, 3)
        m1_r = A.ring("m1", [128, 512], BF16, 4)
        x2_r = A.ring("x2s", [128, 512], BF16, 4)
        G_r = A.ring("G", [128, 512], F32, 4)
        G1_r = A.ring("G1", [128, 512], BF16, 4)
        G2_r = A.ring("G2", [128, 512], BF16, 4)
        carry = A.tile("carry", [128, 16], F32)
        ct1 = A.tile("ct1", [128, 16], F32)
        y_sb = A.tile("y_sb", [128, L], F32)
        ge = {k: A.ring("ge" + k, [128, 512], F32, n_) for k, n_ in (("y", 4), ("a", 2), ("b", 2), ("s", 2))}
        ybank = [banks[7], banks[7]]
        xbanks = Ring(banks[0:4])
        zbanks = Ring(banks[4:6])
        swbank = banks[6]
        for cc in range(4):
            tabs = []

            def mk_tab(d, gl, which, dst):
                u_ = d * 8 + gl
                dg = d * 32 + cc * 8 + gl
                h_ = {}

                def tA():
                    h_["t"] = t_ = tg_t.next()
                    r_ = tg_i.next()
                    act(t_[:], jrow[:, d, :], AF.Identity, [jrow, fcol, magc], [t_], scale=fcol[:, dg:dg + 1], bias=magc[:, 2:3] if which else magc[:, 3:4])
                    act(r_[:], t_[:], AF.Identity, [t_, magc], [r_], bias=magc[:, 0:1])
                    act(r_[:], r_[:], AF.Identity, [r_, magc], [r_], bias=magc[:, 1:2])
                    tt("pool", t_[:], t_[:], r_[:], ALU.subtract, [t_, r_], [t_])

                def tB():
                    act(dst[:, u_, :], h_["t"][:], AF.Sin, [h_["t"]], [dst.sub(u_)], scale=TWO_PI_LO)
                    if which == 1:
                        c512 = 511 if d == 0 else 0
                        c256 = 255 if d == 0 else 256
                        for k_, col in ((0, c512), (1, c256)):
                            cp("pool", ccol[:, u_, k_:k_ + 1], COS[:, u_, col:col + 1], [COS.sub(u_)], [ccol.sub(u_)])
                            ts("pool", scol[:, u_, k_:k_ + 1], SIN[:, u_, col:col + 1], sgn[:, 0:1], None, ALU.mult, None, [SIN.sub(u_), sgn], [scol.sub(u_)])
                return tA, tB

            for d in range(2):
                for gl in range(8):
                    u_ = d * 8 + gl
                    for which, dst in ((0, SIN), (1, COS)):
                        tabs.append(mk_tab(d, gl, which, dst))
                    dcc = d * 4 + cc
                    ts("dve", X1w[:, u_, :], BB1T[:, dcc, :], rowmask[:, gl:gl + 1], None, ALU.mult, None, [BB1T, rowmask], [X1w.sub(u_)])
                    ts("dve", X2w[:, u_, :], BB2T[:, dcc, :], rowmask[:, gl:gl + 1], None, ALU.mult, None, [BB2T, rowmask], [X2w.sub(u_)])
                    memset("pool", W1p[:, u_, :], 0.0, [W1p.sub(u_)])
                    memset("pool", W2p[:, u_, :], 0.0, [W2p.sub(u_)])
                    cp("pool", W1p[:, u_, gl * 16:(gl + 1) * 16], W1d[:, dcc, gl * 16:(gl + 1) * 16], [W1d], [W1p.sub(u_)])
                    cp("pool", W2p[:, u_, gl * 16:(gl + 1) * 16], W2d[:, dcc, gl * 16:(gl + 1) * 16], [W2d], [W2p.sub(u_)])
            for step in range(len(tabs) + 1):
                if step < len(tabs):
                    tabs[step][0]()
                if step >= 1:
                    tabs[step - 1][1]()

            def unit(d, gl, rhs_ap, rhs_bufs, n, tab_lo, init_ap, init_bufs, readout, last_col, kcol, ybk=None, first=False, last=False, post=None):
                u_ = d * 8 + gl
                dg = d * 32 + cc * 8 + gl
                st = {}
                cosv = COS[:, u_, tab_lo:tab_lo + n]
                sinv = SIN[:, u_, tab_lo:tab_lo + n]

                def s0():
                    st["p1"] = p1 = xbanks.next()
                    st["p2"] = p2 = xbanks.next()
                    mm(p1[:, 0:n], X1w[:, u_, :], rhs_ap, True, True, [X1w.sub(u_)] + rhs_bufs, [p1])
                    mm(p2[:, 0:n], X2w[:, u_, :], rhs_ap, True, True, [X2w.sub(u_)] + rhs_bufs, [p2])

                def s1():
                    st["m1"] = m1 = m1_r.next()
                    st["x2"] = x2 = x2_r.next()
                    tt("dve", m1[:, 0:n], st["p1"][:, 0:n], cosv, ALU.mult, [st["p1"], COS.sub(u_)], [m1])
                    tt("dve", x2[:, 0:n], st["p2"][:, 0:n], sinv, ALU.mult, [st["p2"], SIN.sub(u_)], [x2])

                def s2():
                    st["z"] = zb = zbanks.next()
                    mm(zb[:, 0:n], identb[:], st["m1"][:, 0:n], True, False, [identb, st["m1"]], [zb])
                    mm(zb[:, 0:n], identb[:], st["x2"][:, 0:n], False, True, [identb, st["x2"]], [zb])

                def s3():
                    st["G"] = G = G_r.next()
                    z = st["z"]
                    rbc = rcol[:, dg:dg + 1].to_broadcast([128, n])
                    if d == 0:
                        S.op("dve", lambda e: e.tensor_tensor_scan(out=G[:, 0:n], data0=rbc, data1=z[:, 0:n], initial=init_ap, op0=ALU.mult, op1=ALU.add), [z, rcol] + init_bufs, [G])
                    else:
                        S.op("dve", lambda e: e.tensor_tensor_scan(out=G[:, 0:n][:, ::-1], data0=rbc, data1=z[:, 0:n][:, ::-1], initial=init_ap, op0=ALU.mult, op1=ALU.add), [z, rcol] + init_bufs, [G])

                def s4():
                    G = st["G"]
                    mm(swbank[:, u_:u_ + 1], pswap[:], G[:, last_col:last_col + 1], True, True, [pswap, G], [swbank.sub(u_)])
                    act(ct1[:, u_:u_ + 1], swbank[:, u_:u_ + 1], AF.Identity, [swbank.sub(u_), scol.sub(u_)], [ct1.sub(u_)], scale=scol[:, u_, kcol:kcol + 1])
                    if readout:
                        st["G1"] = G1 = G1_r.next()
                        st["G2"] = G2 = G2_r.next()
                        tt("pool", G1[:, 0:n], G[:, 0:n], cosv, ALU.mult, [G, COS.sub(u_)], [G1])
                        tt("pool", G2[:, 0:n], G[:, 0:n], sinv, ALU.mult, [G, SIN.sub(u_)], [G2])

                def s5():
                    G = st["G"]
                    stt(carry[:, u_:u_ + 1], G[:, last_col:last_col + 1], ccol[:, u_, kcol:kcol + 1], ct1[:, u_:u_ + 1], ALU.mult, ALU.add, [G, ccol.sub(u_), ct1.sub(u_)], [carry.sub(u_)])
                    if readout:
                        mm(ybk[:, 0:n], W1p[:, u_, :], st["G1"][:, 0:n], first, False, [W1p.sub(u_), st["G1"]], [ybk])
                        mm(ybk[:, 0:n], W2p[:, u_, :], st["G2"][:, 0:n], False, last, [W2p.sub(u_), st["G2"]], [ybk])
                    if post is not None:
                        post()
                return [s0, s1, s2, s3, s4, s5]

            units = []
            for d in range(2):
                for gl in range(8):
                    lastc = 255 if d == 0 else 0
                    units.append(unit(d, gl, uTc[:, cc, :], [uTc.sub(cc)], 256, 0 if d == 0 else 256, 0.0, [], False, lastc, 1))
            touched = set()
            for i in range(8):
                for d in range(2):
                    tc = i if d == 0 else 7 - i
                    ybk = ybank[d]
                    for gl in range(8):
                        u_ = d * 8 + gl
                        lastc = 511 if d == 0 else 0
                        post = None
                        if gl == 7:
                            def post(tc=tc, ybk=ybk, fresh=(tc not in touched)):
                                ysl = y_sb[:, tc * 512:(tc + 1) * 512]
                                if fresh:
                                    cp("act", ysl, ybk[:], [ybk], [y_sb.sub(tc)])
                                else:
                                    tt("dve", ysl, ysl, ybk[:], ALU.add, [ybk, y_sb.sub(tc)], [y_sb.sub(tc)])
                            touched.add(tc)
                        units.append(unit(d, gl, uT[:, cc, tc * 512:(tc + 1) * 512], [uT.sub((cc, tc))], 512, 0, carry[:, u_:u_ + 1], [carry.sub(u_)], True, lastc, 0, ybk=ybk, first=(gl == 0), last=(gl == 7), post=post))
            NST = 6
            for step in range(len(units) + NST - 1):
                for k_ in range(NST):
                    ui = step - k_
                    if 0 <= ui < len(units):
                        units[ui][k_]()
            gst = [dict() for _ in range(8)]

            def gA(tc):
                sl = slice(tc * 512, (tc + 1) * 512)
                g_ = gst[tc]
                g_["y"] = yv = ge["y"].next()
                g_["a"] = a_ = ge["a"].next()
                stt(yv[:], uT[:, cc, sl], dskT[:, cc:cc + 1], y_sb[:, sl], ALU.mult, ALU.add, [uT.sub((cc, tc)), dskT, y_sb.sub(tc)], [yv])
                tt("dve", a_[:], yv[:], yv[:], ALU.mult, [yv], [a_])

            def gB(tc):
                g_ = gst[tc]
                a_, yv = g_["a"], g_["y"]
                g_["b"] = b_ = ge["b"].next()
                ts("pool", a_[:], a_[:], 0.044715, 1.0, ALU.mult, ALU.add, [a_], [a_])
                tt("pool", b_[:], a_[:], yv[:], ALU.mult, [a_, yv], [b_])

            def gC(tc):
                g_ = gst[tc]
                g_["s"] = s_ = ge["s"].next()
                act(s_[:], g_["b"][:], AF.Sigmoid, [g_["b"]], [s_], scale=1.5957691216057308)

            def gD(tc):
                sl = slice(tc * 512, (tc + 1) * 512)
                g_ = gst[tc]
                tt("dve", uT[:, cc, sl], g_["y"][:], g_["s"][:], ALU.mult, [g_["y"], g_["s"]], [uT.sub((cc, tc))])
            gfs = [gA, gB, gC, gD]
            for step in range(8 + 3):
                for k_ in range(4):
                    tc = step - k_
                    if 0 <= tc < 8:
                        gfs[k_](tc)
            for tc in range(8):
                dma("act", yg_s[:, cc, tc * 512:(tc + 1) * 512], uT[:, cc, tc * 512:(tc + 1) * 512], [uT.sub((cc, tc))], [yg_s.sub(tc)])
        barrier()
        A.release(m)

    def phase_B(b):
        m = A.mark()
        w_rest = A.tile("w_rest", [128, 8, 3584], BF16)
        w_glu = A.tile("w_glu", [128, 4, 512], BF16)
        w_sbr = A.tile("w_sbr", [128, 4, D], BF16)
        w_cbr = A.tile("w_cbr", [128, 4, D], BF16)
        w_o = A.tile("w_o", [128, 8, D], BF16)
        for kc in range(8):
            for h_ in range(2):
                lo = h_ * 1792
                dma("pool", w_rest[:, kc, lo:lo + 1792], w_in_d[kc * 128:(kc + 1) * 128, 512 + lo:512 + lo + 1792], [], [w_rest.sub(kc)])
        dma("pool", w_glu[:], w_glu_d.rearrange("(kc p) n -> p kc n", p=128), [], [w_glu])
        dma("pool", w_sbr[:], w_ssm_br_d.rearrange("(kc p) n -> p kc n", p=128), [], [w_sbr])
        dma("pool", w_cbr[:], w_conv_br_d.rearrange("(kc p) n -> p kc n", p=128), [], [w_cbr])
        dma("pool", w_o[:], w_o_d.rearrange("(kc p) n -> p kc n", p=128), [], [w_o])
        diag_r = A.ring("diag", [128, 128], F32, 2)
        for hh in range(2):
            pg = bank_rr.next()
            for q in range(4):
                j = hh * 4 + q
                dg_ = diag_r.next()
                ts("dve", dg_[:], identf[:], modT[:, 16 + j, b:b + 1], None, ALU.mult, None, [identf, modT], [dg_])
                mm(pg[:, q * 128:(q + 1) * 128], onesf[:], dg_[:], True, True, [onesf, dg_], [pg])
            for kc in range(8):
                tt("dve", w_o[:, kc, hh * 512:(hh + 1) * 512], w_o[:, kc, hh * 512:(hh + 1) * 512], pg[:], ALU.mult, [w_o, pg], [w_o])
        xt_ring = A.ring("xt", [128, D], F32, 4)
        tiles = (xt_ring, A.ring("xn", [128, D], BF16, 3), None, A.ring("ss", [128, 4], F32, 4))
        hx_r = A.ring("hxT", [128, 8, 512], BF16, 1)
        yg_r = A.ring("ygT", [128, 4, 512], BF16, 2)
        v_r = A.ring("v_sb", [128, 512], F32, 2)
        yc_r = A.ring("yc", [128, 512], F32, 2)
        convT = A.tile("convT", [128, 4, 512], BF16)
        ssmT = A.tile("ssmT", [128, 4, 512], BF16)
        sg_r = A.ring("sg", [128, 512], F32, 2)
        sgs_r = A.ring("sgs", [128, 512], F32, 2)
        sgc_r = A.ring("sgc", [128, 512], F32, 2)
        mgT = A.tile("mgT", [128, 8, 512], BF16)
        xo_r = xt_ring

        def wcol(c0):
            return c0 - 512

        for tg in range(8):
            base = b * L + tg * 512
            conv_step(2)
            hxT = hx_r.next()
            ygT = yg_r.next()
            dma("sp", ygT[:], yg_s[:, :, tg * 512:(tg + 1) * 512], [yg_s.sub(tg)], [ygT])
            norm_tiles(tiles, lambda t_: x_d[base + t_ * 128: base + (t_ + 1) * 128, :], 4, b, hxT, gsm, 0)

            def proj(c0):
                pb = bank_rr.next()
                for kc in range(8):
                    mm(pb[:], w_rest[:, kc, wcol(c0):wcol(c0) + 128], hxT[:, kc, :], kc == 0, kc == 7, [w_rest.sub(kc), hxT], [pb])
                return pb
            for cc in range(4):
                pv = proj(512 + cc * 128)
                pgb = proj(1024 + cc * 128)
                pgc = proj(1536 + cc * 128)
                v_sb = v_r.next()
                zc = v_sb
                yc = yc_r.next()
                cp("act", v_sb[:], pv[:], [pv], [v_sb])
                tt("dve", zc[:], pgc[:], v_sb[:], ALU.mult, [pgc, v_sb], [zc])
                ts("dve", yc[:], zc[:], cwT[:, 1, cc:cc + 1], cbT[:, cc:cc + 1], ALU.mult, ALU.add, [zc, cwT, cbT], [yc])
                z3 = zc[:].rearrange("p (r t) -> p r t", t=64)
                y3 = yc[:].rearrange("p (r t) -> p r t", t=64)
                stt(y3[:, :, 1:64], z3[:, :, 0:63], cwT[:, 0, cc:cc + 1], y3[:, :, 1:64], ALU.mult, ALU.add, [zc, cwT, yc], [yc])
                stt(y3[:, :, 0:63], z3[:, :, 1:64], cwT[:, 2, cc:cc + 1], y3[:, :, 0:63], ALU.mult, ALU.add, [zc, cwT, yc], [yc])
                tt("dve", convT[:, cc, :], pgb[:], yc[:], ALU.mult, [pgb, yc], [convT.sub(cc)])
            for oc in range(4):
                pb = bank_rr.next()
                for kc in range(4):
                    mm(pb[:], w_glu[:, kc, oc * 128:(oc + 1) * 128], ygT[:, kc, :], kc == 0, kc == 3, [w_glu, ygT], [pb])
                sg = sg_r.next()
                act(sg[:], pb[:], AF.Sigmoid, [pb], [sg])
                tt("pool", ssmT[:, oc, :], ygT[:, oc, :], sg[:], ALU.mult, [ygT, sg], [ssmT.sub(oc)])
            for oc in range(8):
                pgs = proj(2048 + oc * 128)
                pgcg = proj(3072 + oc * 128)
                pys = bank_rr.next()
                for kc in range(4):
                    mm(pys[:], w_sbr[:, kc, oc * 128:(oc + 1) * 128], ssmT[:, kc, :], kc == 0, kc == 3, [w_sbr, ssmT.sub(kc)], [pys])
                pyc = bank_rr.next()
                for kc in range(4):
                    mm(pyc[:], w_cbr[:, kc, oc * 128:(oc + 1) * 128], convT[:, kc, :], kc == 0, kc == 3, [w_cbr, convT.sub(kc)], [pyc])
                sgs = sgs_r.next()
                sgc = sgc_r.next()
                ms = sgs
                mc_ = sgc
                act(sgs[:], pgs[:], AF.Sigmoid, [pgs], [sgs])
                act(sgc[:], pgcg[:], AF.Sigmoid, [pgcg], [sgc])
                tt("dve", ms[:], pys[:], sgs[:], ALU.mult, [pys, sgs], [ms])
                tt("dve", mc_[:], pyc[:], sgc[:], ALU.mult, [pyc, sgc], [mc_])
                tt("pool", mgT[:, oc, :], ms[:], mc_[:], ALU.add, [ms, mc_], [mgT.sub(oc)])
            for t_ in range(4):
                xo = xo_r.next()
                r0 = base + t_ * 128
                dma("sp", xo[:], x_d[r0:r0 + 128, :], [], [xo])
                for hh in range(2):
                    po = bank_rr.next()
                    for kc in range(8):
                        mm(po[:], mgT[:, kc, t_ * 128:(t_ + 1) * 128], w_o[:, kc, hh * 512:(hh + 1) * 512], kc == 0, kc == 7, [mgT.sub(kc), w_o], [po])
                    tt("dve", xo[:, hh * 512:(hh + 1) * 512], xo[:, hh * 512:(hh + 1) * 512], po[:], ALU.add, [po, xo], [xo])
                dma("act", x1_s[r0:r0 + 128, :], xo[:], [xo], [x1_s.sub(r0 // 128)])
        barrier()
        A.release(m)

    stage = dbg.get("stage", 99)
    for b in range(NBL):
        m_as = A.mark()
        AS["uT"] = uT = A.tile("uT", [128, 4, L], BF16)
        AS["uTc"] = A.tile("uTc", [128, 4, CTXL], BF16)
        phase_A(b)
        if "u" in dbg and b == 0:
            o = dbg_tensor("dbg_u", [128, 4 * L])
            uf = A.tile("uf", [128, 4 * L // 8], F32)
            for i in range(8):
                n_ = 4 * L // 8
                cp("dve", uf[:], uT[:].rearrange("p a b -> p (a b)")[:, i * n_:(i + 1) * n_], [uT.sub((c_, t_)) for c_ in range(4) for t_ in range(8)], [uf])
                dma("sp", o[:, i * n_:(i + 1) * n_], uf[:], [uf], [])
            barrier()
        if stage < 2:
            break
        phase_S(b)
        if "yg" in dbg and b == 0:
            o = dbg_tensor("dbg_yg", [128, 4 * L])
            uf = A.tile("uf2", [128, 4 * L // 8], F32)
            for i in range(8):
                n_ = 4 * L // 8
                cp("dve", uf[:], uT[:].rearrange("p a b -> p (a b)")[:, i * n_:(i + 1) * n_], [uT.sub((c_, t_)) for c_ in range(4) for t_ in range(8)], [uf])
                dma("sp", o[:, i * n_:(i + 1) * n_], uf[:], [uf], [])
            barrier()
        if stage < 3:
            break
        A.release(m_as)
        phase_B(b)
        if stage < 4:
            break
    if "x1" in dbg:
        o = dbg_tensor("dbg_x1", [L, D])
        xr = A.ring("xdbg", [128, D], F32, 2)
        for i in range(L // 128):
            t_ = xr.next()
            dma("sp", t_[:], x1_s[i * 128:(i + 1) * 128, :], [x1_s.sub(i)], [t_])
            dma("sp", o[i * 128:(i + 1) * 128, :], t_[:], [t_], [])


    SP_ENG = mybir.EngineType.SP
    SIGMAX = 1.0 / (1.0 + math.exp(-1.702 * 7.0))
    AX = mybir.AxisListType

    def make_row(dst, colfn, diag_r):
        for hh in range(2):
            pg = bank_rr.next()
            for q in range(4):
                j = hh * 4 + q
                dg_ = diag_r.next()
                col, cb = colfn(j)
                ts("dve", dg_[:], identf[:], col, None, ALU.mult, None, [identf, cb], [dg_])
                mm(pg[:, q * 128:(q + 1) * 128], onesf[:], dg_[:], True, True, [onesf, dg_], [pg])
            cp("act", dst[:, hh * 512:(hh + 1) * 512], pg[:], [pg], [dst])

    def phase_moe():
        bgT_s = dscr("bgT_s", [NE * 128, 24], F32)
        conv_step(len(conv_jobs))
        m_moe = A.mark()
        GK = [A.tile("GK%d" % k, [128, 64], F32) for k in range(TOPK)]
        DK = [A.tile("DK%d" % k, [128, 64], I32) for k in range(TOPK)]
        GT = A.tile("GT", [128, 64, NE], F32)
        IDX = A.tile("IDX", [128, NBLK], I32)
        IDX3 = A.tile("IDX3", [128, NBLK], I32)
        bd = A.tile("bd", [NE, D], F32)
        dma("sp", bd[:], b_down_d, [], [bd])
        diag_r = A.ring("diag", [128, 128], F32, 2)
        m_r = A.mark()
        zt = A.tile("zt", [128, 4, D], BF16)
        memset("pool", zt[:], 0.0, [zt])
        xbA = xb_s.sub("all")
        zsubs = [xb_s.sub(("z", i)) for i in range(CAP // 512)]
        for i in range(CAP // 512):
            dma("sp", xb_s[i * 512:(i + 1) * 512, :].rearrange("(n p) d -> p n d", p=128), zt[:], [zt], [zsubs[i]])
        wr = A.tile("wr", [128, 8, NE], F32)
        dma("sp", wr[:], w_router_d.rearrange("(kc p) e -> p kc e", p=128), [], [wr])
        brow = A.tile("brow", [128, NE], F32)
        dma("sp", brow[:], bass.AP(tensor=b_router_d.tensor, offset=0, ap=[[0, 128], [1, NE]]), [], [brow])
        tri = A.tile("tri", [128, 128], F32)
        memset("pool", tri[:], 1.0, [tri])
        S.op("pool", lambda e: e.affine_select(out=tri[:], in_=tri[:], pattern=[[1, 128]], compare_op=ALU.is_gt, fill=0.0, base=0, channel_multiplier=-1), [tri], [tri])
        bg = A.tile("bg", [NE, 2 * D], F32)
        dma("sp", bg[:], b_gu_d, [], [bg])
        bgT = A.tile("bgT", [128, NE, 24], F32)
        for fc in range(16):
            pb = bank_rr.next()
            tr(pb[:, 0:NE], bg[0:NE, fc * 128:(fc + 1) * 128], identf[0:NE, 0:NE], [bg, identf], [pb])
            if fc < 8:
                ts("dve", bgT[:, :, fc], pb[:, 0:NE], 1.702, None, ALU.mult, None, [pb], [bgT])
                cp("act", bgT[:, :, 8 + fc], pb[:, 0:NE], [pb], [bgT])
            else:
                ts("dve", bgT[:, :, 8 + fc], pb[:, 0:NE], 1.0, None, ALU.add, None, [pb], [bgT])
        dma("sp", bgT_s[:, :].rearrange("(e p) c -> p e c", p=128), bgT[:], [bgT], [bgT_s])
        gs_row = [A.tile("gs_row%d" % b, [128, D], F32) for b in range(NBL)]
        sh_row = [A.tile("sh_row%d" % b, [128, D], F32) for b in range(NBL)]
        for b in range(NBL):
            make_row(gs_row[b], lambda j, b=b: (gsf[:, j, b:b + 1], gsf), diag_r)
            make_row(sh_row[b], lambda j, b=b: (modT[:, 24 + j, b:b + 1], modT), diag_r)
        LG = A.tile("LG", [128, 64, NE], F32)
        MK = A.tile("MK", [128, 64, NE], F32)
        RK = A.tile("RK", [128, 64, NE], F32)
        M8 = A.tile("M8", [128, 64, 8], F32)
        Macc = A.tile("Macc", [128, NE], F32)
        xt_r = A.ring("xt", [128, D], F32, 4)
        hf_r = A.ring("hf", [128, D], F32, 3)
        hb_r = A.ring("hbt", [128, D], BF16, 4)
        hlo_r = A.ring("hlo", [128, D], BF16, 3)
        hTh_r = A.ring("hTh", [128, 8, 128], BF16, 2)
        hTl_r = A.ring("hTl", [128, 8, 128], BF16, 2)
        wr_hi = A.tile("wr_hi", [128, 8, NE], BF16)
        wr_lo = A.tile("wr_lo", [128, 8, NE], BF16)
        cp("dve", wr_hi[:], wr[:], [wr], [wr_hi])
        tt("dve", wr_lo[:], wr[:], wr_hi[:], ALU.subtract, [wr, wr_hi], [wr_lo])
        ss_r = A.ring("ss", [128, 8], F32, 5)
        E_r = A.ring("E", [128, NE], F32, 2)
        CS = A.tile("CS", [128, 64, NE], F32)
        CS2 = A.tile("CS2", [128, 64, NE], F32)

        def r_tile(i):
            b = i // 32
            st = {}

            def r0():
                st["xt"] = xt = xt_r.next(); st["hf"] = hf_r.next(); st["hbt"] = hbt = hb_r.next(); st["ss"] = ss = ss_r.next()
                dma("sp", xt[:], x1_s[i * 128:(i + 1) * 128, :], [x1_s.sub(i)], [xt])
                memset("pool", ss[:], 0.0, [ss])
                act(hbt[:], xt[:], AF.Square, [xt, ss], [hbt, ss], accum=ss[:, 0:1])
                act(ss[:, 1:2], ss[:, 0:1], AF.Ln, [ss], [ss], scale=1.0 / D, bias=epsc[:, 0:1])
                act(ss[:, 2:3], ss[:, 1:2], AF.Exp, [ss], [ss], scale=-0.5)

            def r0b():
                xt, hf, ss = st["xt"], st["hf"], st["ss"]
                stt(hf[:], xt[:], ss[:, 2:3], gs_row[b][:], ALU.mult, ALU.mult, [xt, ss, gs_row[b]], [hf])
                tt("pool", hf[:], hf[:], sh_row[b][:], ALU.add, [hf, sh_row[b]], [hf])

            def r0c():
                hf, hbt = st["hf"], st["hbt"]
                cp("act", hbt[:], hf[:], [hf], [hbt])
                dma("act", hb_s[i * 128:(i + 1) * 128, :], hbt[:], [hbt], [hb_s.sub(i)])
                st["hlo"] = hlo = hlo_r.next()
                tt("dve", hlo[:], hf[:], hbt[:], ALU.subtract, [hf, hbt], [hlo])
                st["pA"] = pA = bank_rr.next(); st["pB"] = pB = bank_rr.next()
                pAb = pA.h.bitcast(BF16); pBb = pB.h.bitcast(BF16)
                for kc in range(8):
                    tr(pAb[:, kc * 128:(kc + 1) * 128], hbt[:, kc * 128:(kc + 1) * 128], identb[:], [hbt, identb], [pA])
                for kc in range(8):
                    tr(pBb[:, kc * 128:(kc + 1) * 128], hlo[:, kc * 128:(kc + 1) * 128], identb[:], [hlo, identb], [pB])

            def r1():
                st["hTh"] = hTh = hTh_r.next(); st["hTl"] = hTl = hTl_r.next()
                cp("act", hTh[:].rearrange("p a t -> p (a t)"), st["pA"].h.bitcast(BF16)[:, 0:1024], [st["pA"]], [hTh])
                cp("dve", hTl[:].rearrange("p a t -> p (a t)"), st["pB"].h.bitcast(BF16)[:, 0:1024], [st["pB"]], [hTl])
                st["pl"] = pl = bank_rr.next()
                terms = [(hTh, wr_hi), (hTl, wr_hi), (hTh, wr_lo)]
                n_ = 0
                for (hx_, wx_) in terms:
                    for kc in range(8):
                        mm(pl[:, 0:NE], hx_[:, kc, :], wx_[:, kc, :], n_ == 0, n_ == 23, [hx_, wx_], [pl])
                        n_ += 1

            def r2():
                lgi = LG.sub(i); mki = MK.sub(i)
                tt("dve", LG[:, i, :], st["pl"][:, 0:NE], brow[:], ALU.add, [st["pl"], brow], [lgi])
                S.op("dve", lambda e: e.max(out=M8[:, i, :], in_=LG[:, i, :]), [lgi], [M8.sub(i)])
                ts("dve", MK[:, i, :], LG[:, i, :], M8[:, i, 3:4], None, ALU.is_ge, None, [lgi, M8.sub(i)], [mki])
                st["pr"] = pr = bank_rr.next()
                mm(pr[:, 0:NE], tri[:], MK[:, i, :], True, True, [tri, mki], [pr])
                mm(pr[:, NE:2 * NE], onesf[:], MK[:, i, :], True, True, [onesf, mki], [pr])

            def r3():
                cp("act", RK[:, i, :], st["pr"][:, 0:NE], [st["pr"]], [RK.sub(i)])
                cp("act", CS[:, i, :], st["pr"][:, NE:2 * NE], [st["pr"]], [CS.sub(i)])
            return [r0, r0b, r0c, r1, r2, r3]

        rt = [r_tile(i) for i in range(64)]
        for step in range(64 + 5):
            for k_ in range(6):
                ui = step - k_
                if 0 <= ui < 64:
                    rt[ui][k_]()
        allsub = lambda T_: [T_.sub(i) for i in range(64)]
        OH = A.tile("OH", [128, 64, NE], F32)
        PR = A.tile("PR", [128, 64, NE], F32)
        rsum = A.tile("rsum", [128, 64], F32)
        tt("dve", OH[:], LG[:], M8[:, :, 0:1].to_broadcast([128, 64, NE]), ALU.subtract, allsub(LG) + allsub(M8), [OH])
        act(OH[:], OH[:], AF.Exp, [OH], [OH])
        tt("dve", OH[:], OH[:], MK[:], ALU.mult, [OH] + allsub(MK), [OH])
        S.op("dve", lambda e: e.reduce_sum(out=rsum[:], in_=OH[:], axis=AX.X), [OH], [rsum])
        S.op("dve", lambda e: e.reciprocal(out=rsum[:], in_=rsum[:]), [rsum], [rsum])
        tt("dve", GT[:], OH[:], rsum[:].rearrange("p (a o) -> p a o", o=1).to_broadcast([128, 64, NE]), ALU.mult, [OH, rsum], allsub(GT))
        src, dst = CS, CS2
        first = True
        for sh in (1, 2, 4, 8, 16, 32):
            R_ = allsub(src) if first else [src]
            cp("pool", dst[:, 0:sh, :], src[:, 0:sh, :], R_, [dst])
            tt("dve", dst[:, sh:64, :], src[:, sh:64, :], src[:, 0:64 - sh, :], ALU.add, R_, [dst])
            src, dst = dst, src
            first = False
        PFX = src
        tt("dve", RK[:, 1:64, :], RK[:, 1:64, :], PFX[:, 0:63, :], ALU.add, allsub(RK) + [PFX], [RK])
        sm = {k: A.tile("sm" + k, [128, NE], F32) for k in ("nb", "pad", "pe", "ps", "one")}
        smi = A.tile("smi", [128, NE], I32)

        class _C:
            pass
        cntp = _C()
        cntp_ap = PFX[:, 63, :]
        ts("dve", sm["nb"][:], cntp_ap, 1.0 / 128, 0.49609375, ALU.mult, ALU.add, [PFX], [sm["nb"]])
        cp("pool", smi[:], sm["nb"][:], [sm["nb"]], [smi])
        cp("pool", sm["nb"][:], smi[:], [smi], [sm["nb"]])
        ts("dve", sm["pad"][:], sm["nb"][:], 128.0, None, ALU.mult, None, [sm["nb"]], [sm["pad"]])
        memset("pool", sm["one"][:], 1.0, [sm["one"]])
        S.op("dve", lambda e: e.tensor_tensor_scan(out=sm["pe"][:], data0=sm["one"][:], data1=sm["pad"][:], initial=0.0, op0=ALU.mult, op1=ALU.add), [sm["one"], sm["pad"]], [sm["pe"]])
        tt("dve", sm["ps"][:], sm["pe"][:], sm["pad"][:], ALU.subtract, [sm["pe"], sm["pad"]], [sm["ps"]])
        tt("dve", RK[:], RK[:], sm["ps"][:].rearrange("p (o e) -> p o e", o=1).to_broadcast([128, 64, NE]), ALU.add, [RK, sm["ps"]], [RK])
        DKf = A.tile("DKf", [128, 64], F32)
        for k in range(TOPK):
            tt("dve", OH[:], LG[:], M8[:, :, k:k + 1].to_broadcast([128, 64, NE]), ALU.is_equal, allsub(LG) + allsub(M8), [OH])
            tt("dve", PR[:], OH[:], RK[:], ALU.mult, [OH, RK], [PR])
            S.op("dve", lambda e: e.reduce_sum(out=DKf[:], in_=PR[:], axis=AX.X), [PR], [DKf])
            cp("dve", DK[k][:], DKf[:], [DKf], [DK[k]])
            tt("dve", PR[:], OH[:], GT[:], ALU.mult, [OH] + allsub(GT), [PR])
            S.op("dve", lambda e, k=k: e.reduce_sum(out=GK[k][:], in_=PR[:], axis=AX.X), [PR], [GK[k]])
        bs = A.tile("bs", [128, NBLK], F32)
        be = A.tile("be", [128, NBLK], F32)
        chg = A.tile("chg", [128, NBLK], F32)
        pcol = A.tile("pcol", [128, 1], F32)
        S.op("pool", lambda e: e.iota(bs[:], pattern=[[128, NBLK]], base=0, channel_multiplier=0, allow_small_or_imprecise_dtypes=True), [], [bs])
        S.op("pool", lambda e: e.iota(pcol[:], pattern=[[0, 1]], base=0, channel_multiplier=1, allow_small_or_imprecise_dtypes=True), [], [pcol])
        memset("pool", be[:], 0.0, [be])
        for e_ in range(NE):
            stt(be[:], bs[:], sm["pe"][:, e_:e_ + 1], be[:], ALU.is_ge, ALU.add, [bs, sm["pe"], be], [be])
        ts("dve", be[:], be[:], float(NE - 1), None, ALU.min, None, [be], [be])
        memset("pool", chg[:], 0.0, [chg])
        tt("dve", chg[:, 2:NBLK], be[:, 2:NBLK], be[:, 0:NBLK - 2], ALU.is_equal, [be, chg], [chg])
        ts("dve", bs[:], be[:], 128.0, pcol[:, 0:1], ALU.mult, ALU.add, [be, pcol, bs], [bs])
        stt(bs[:], chg[:], 1.0e6, bs[:], ALU.mult, ALU.add, [chg, bs], [bs])
        cp("dve", IDX[:], bs[:], [bs], [IDX])
        memset("pool", chg[:], 0.0, [chg])
        tt("dve", chg[:, 3:NBLK], be[:, 3:NBLK], be[:, 0:NBLK - 3], ALU.is_equal, [be, chg], [chg])
        ts("dve", bs[:], be[:], 128.0, pcol[:, 0:1], ALU.mult, ALU.add, [be, pcol, bs], [bs])
        stt(bs[:], chg[:], 1.0e6, bs[:], ALU.mult, ALU.add, [chg, bs], [bs])
        cp("dve", IDX3[:], bs[:], [bs], [IDX3])
        if "route" in dbg:
            o = dbg_tensor("dbg_route", [128, 64 * NE * 2 + 64 * 4 * 2 + 2 * NBLK])
            dma("sp", o[:, 0:64 * NE], LG[:].rearrange("p a e -> p (a e)"), allsub(LG), [])
            dma("sp", o[:, 64 * NE:2 * 64 * NE], GT[:].rearrange("p a e -> p (a e)"), allsub(GT), [])
            for k in range(TOPK):
                dma("sp", o[:, 2 * 64 * NE + k * 64:2 * 64 * NE + (k + 1) * 64], DKf[:] if False else GK[k][:], [GK[k]], [])
            dkf2 = A.tile("dkf2", [128, 4, 64], F32)
            for k in range(TOPK):
                cp("dve", dkf2[:, k, :], DK[k][:], [DK[k]], [dkf2])
            dma("sp", o[:, 2 * 64 * NE + 256:2 * 64 * NE + 512], dkf2[:].rearrange("p a b -> p (a b)"), [dkf2], [])
            dma("sp", o[0:1, 2 * 64 * NE + 512:2 * 64 * NE + 512 + NBLK], be[0:1, :], [be], [])
            dma("sp", o[0:1, 2 * 64 * NE + 512 + NBLK:2 * 64 * NE + 512 + 2 * NBLK], chg[0:1, :], [chg], [])
        if dbg.get("moe_stop") == "R":
            return
        regs_c = {}

        def creg(e):
            if "c" not in regs_c:
                regs_c["c"] = e.to_reg(CAP - 1)
            return regs_c["c"]
        hs_r = A.ring("hsc", [128, D], BF16, 4)
        for i in range(64):
            ht = hs_r.next()
            dma("sp", ht[:], hb_s[i * 128:(i + 1) * 128, :], [hb_s.sub(i)], [ht])
            for k in range(TOPK):
                S.op("pool", lambda e, ht=ht, k=k, i=i: e.indirect_dma_start(
                    out=xb_s[:, :], out_offset=bass.IndirectOffsetOnAxis(ap=DK[k][:, i:i + 1], axis=0), in_=ht[:], in_offset=None,
                    bounds_check=creg(e), oob_is_err=False), [ht, DK[k], xbA] + zsubs, [], dma=True)
        memset("pool", bar_t[:], 0.0, [xbA])
        barrier()
        A.release(m_r)
        if dbg.get("moe_stop") == "S":
            return
        m_e = A.mark()
        wgu = [A.tile("wgu%d" % s_, [128, 8, 2 * D], BF16) for s_ in range(3)]
        wdn = [A.tile("wdn%d" % s_, [128, 8, D], BF16) for s_ in range(2)]
        bcol = [A.tile("bcol%d" % s_, [128, 24], F32) for s_ in range(3)]
        xb_r = A.ring("xbt", [128, D], BF16, 4)
        hTb_r = A.ring("hTb", [128, 8, 128], BF16, 2)
        gcl_r = A.ring("gcl", [128, 256], F32, 4)
        sig_r = A.ring("sig", [128, 256], F32, 4)
        u1_r = A.ring("u1", [128, 256], F32, 4)
        actT_r = A.ring("actT", [128, 8, 128], BF16, 2)
        yo_r = A.ring("yo", [128, D], F32, 2)
        tbanks = Ring([banks[0], banks[7]])
        gbanks = Ring(banks[1:5])
        dbanks = [banks[5], banks[6]]
        regs = {}

        def breg(e):
            if "w" not in regs:
                regs["w"] = e.to_reg(NE * 128 - 1)
            return regs["w"]
        conv_gu = [wgu_s.sub(e_) for e_ in range(NE)]
        conv_dn = [wdn_s.sub(e_) for e_ in range(NE)]

        def wload_gu(b_):
            s3 = b_ % 3
            S.op("pool", lambda e: e.indirect_dma_start(out=wgu[s3][:].rearrange("p a n -> p (a n)"), out_offset=None, in_=wgu_s[:, :],
                 in_offset=bass.IndirectOffsetOnAxis(ap=IDX3[:, b_:b_ + 1], axis=0), bounds_check=breg(e), oob_is_err=False), [IDX3] + conv_gu, [wgu[s3]], dma=True)
            S.op("pool", lambda e: e.indirect_dma_start(out=bcol[s3][:], out_offset=None, in_=bgT_s[:, :],
                 in_offset=bass.IndirectOffsetOnAxis(ap=IDX3[:, b_:b_ + 1], axis=0), bounds_check=breg(e), oob_is_err=False), [IDX3, bgT_s], [bcol[s3]], dma=True)

        def wload_dn(b_):
            s_ = b_ % 2
            S.op("pool", lambda e: e.indirect_dma_start(out=wdn[s_][:].rearrange("p a n -> p (a n)"), out_offset=None, in_=wdn_s[:, :],
                 in_offset=bass.IndirectOffsetOnAxis(ap=IDX[:, b_:b_ + 1], axis=0), bounds_check=breg(e), oob_is_err=False), [IDX] + conv_dn, [wdn[s_]], dma=True)

        def xload(b_):
            t_ = xb_r.next()
            dma("sp", t_[:], xb_s[b_ * 128:(b_ + 1) * 128, :], [xbA], [t_])
            return t_

        def down(b_, actT):
            s_ = b_ % 2
            for fc in range(8):
                for hh in range(2):
                    mm(dbanks[hh][:], actT[:, fc, :], wdn[s_][:, fc, hh * 512:(hh + 1) * 512], fc == 0, fc == 7, [actT, wdn[s_]], [dbanks[hh]])
            yo = yo_r.next()
            cp("act", yo[:, 0:512], dbanks[0][:], [dbanks[0]], [yo])
            cp("act", yo[:, 512:1024], dbanks[1][:], [dbanks[1]], [yo])
            dma("act", yb_s[b_ * 128:(b_ + 1) * 128, :], yo[:], [yo], [yb_s.sub(b_)])

        nblk = dbg.get("nblk", NBLK)
        ebis = dbg.get("ebis", 9)
        if ebis == 0:
            return
        wload_gu(0); wload_gu(1); wload_gu(2)
        wload_dn(0); wload_dn(1)
        if ebis == 1:
            o = dbg_tensor("dbg_w", [128, 24 + 2048 + 1024])
            wtmp = A.tile("wtmp", [128, 2048 + 1024], F32)
            cp("dve", wtmp[:, 0:2048], wgu[0][:, 3, :], [wgu[0]], [wtmp])
            cp("dve", wtmp[:, 2048:3072], wdn[1][:, 5, :], [wdn[1]], [wtmp])
            dma("sp", o[:, 0:24], bcol[0][:], [bcol[0]], [])
            dma("sp", o[:, 24:24 + 3072], wtmp[:], [wtmp], [])
            return
        def transp(xt_):
            tb = tbanks.next()
            tbb = tb.h.bitcast(BF16)
            for kc in range(8):
                tr(tbb[:, kc * 128:(kc + 1) * 128], xt_[:, kc * 128:(kc + 1) * 128], identb[:], [xt_, identb], [tb])
            hT_ = hTb_r.next()
            cp("act", hT_[:].rearrange("p a t -> p (a t)"), tbb[:, 0:1024], [tb], [hT_])
            return hT_

        xq = {0: xload(0)}
        if nblk > 1:
            xq[1] = xload(1)
        hT_next = transp(xq.pop(0))
        prev = None
        for b_ in range(nblk):
            s_ = b_ % 2
            hT = hT_next
            if b_ + 2 < nblk:
                xq[b_ + 2] = xload(b_ + 2)
            if b_ + 1 < nblk:
                hT_next = transp(xq.pop(b_ + 1))
            if ebis == 2:
                continue
            actT = actT_r.next()
            pend_fin = None
            for pg_ in range(4):
                gb_ = gbanks.next()
                fcs = (2 * pg_, 2 * pg_ + 1, 8 + 2 * pg_, 9 + 2 * pg_)
                for q, fc in enumerate(fcs):
                    for kc in range(8):
                        mm(gb_[:, q * 128:(q + 1) * 128], wgu[b_ % 3][:, kc, fc * 128:(fc + 1) * 128], hT[:, kc, :], kc == 0, kc == 7, [wgu[b_ % 3], hT], [gb_])
                if ebis == 3:
                    continue
                gcl = gcl_r.next(); sig = sig_r.next(); u1 = u1_r.next()
                for q in range(2):
                    fc = 2 * pg_ + q
                    sl = slice(q * 128, (q + 1) * 128)
                    ts("dve", gcl[:, sl], gb_[:, sl], bcol[b_ % 3][:, 8 + fc:9 + fc], 7.0, ALU.add, ALU.min, [gb_, bcol[b_ % 3]], [gcl])
                    ts("dve", u1[:, sl], gb_[:, 256 + q * 128:256 + (q + 1) * 128], bcol[b_ % 3][:, 16 + fc:17 + fc], -6.0, ALU.add, ALU.max, [gb_, bcol[b_ % 3]], [u1])
                act(sig[:], gcl[:], AF.Sigmoid, [gcl], [sig], scale=1.702)

                def fin(pg_=pg_, gcl=gcl, sig=sig, u1=u1):
                    tt("dve", sig[:], sig[:], gcl[:], ALU.mult, [sig, gcl], [sig])
                    stt(actT[:, 2 * pg_:2 * pg_ + 2, :].rearrange("p a t -> p (a t)"), u1[:], 8.0, sig[:], ALU.min, ALU.mult, [u1, sig], [actT])
                if pend_fin is not None:
                    pend_fin()
                pend_fin = fin
            if pend_fin is not None:
                pend_fin()
                pend_fin = None
            if ebis <= 4 or ebis >= 40:
                continue
            if prev is not None:
                down(*prev)
            prev = (b_, actT)
            if b_ >= 1 and b_ + 2 < nblk:
                wload_gu(b_ + 2)
            if b_ >= 1 and b_ + 1 < nblk:
                wload_dn(b_ + 1)
        if prev is not None:
            down(*prev)
        ybJ = yb_s.sub("join")
        memset("pool", bar_t[:], 0.0, [ybJ])
        S.op("pool", lambda e: e.memset(bar_t[:], 0.0), [yb_s.sub(b_) for b_ in range(nblk)], [ybJ])
        barrier()
        A.release(m_e)
        if dbg.get("moe_stop") == "E":
            return
        g5_row = [A.tile("g5_row%d" % b, [128, D], F32) for b in range(NBL)]
        gfin_row = A.tile("gfin_row", [128, D], F32)
        for b in range(NBL):
            make_row(g5_row[b], lambda j, b=b: (modT[:, 40 + j, b:b + 1], modT), diag_r)
        make_row(gfin_row, lambda j: (gfinT[:, j:j + 1], gfinT), diag_r)
        Y_r = A.ring("Y", [128, D], F32, 12)
        acc_r = A.ring("acc", [128, D], F32, 4)
        x1_r = A.ring("x1t", [128, D], F32, 4)
        gT_r = A.ring("gT", [NE, 128], F32, 3)
        ss_r = A.ring("ssg", [128, 4], F32, 4)
        sq_r = A.ring("sqg", [128, D], BF16, 1)

        def g_tile(i):
            b = i // 32
            st = {}

            def g0():
                st["x1t"] = x1t = x1_r.next()
                dma("sp", x1t[:], x1_s[i * 128:(i + 1) * 128, :], [x1_s.sub(i)], [x1t])
                st["Ys"] = Ys = []
                for k in range(TOPK):
                    Y = Y_r.next()
                    S.op("pool", lambda e, Y=Y, k=k: e.indirect_dma_start(
                        out=Y[:], out_offset=None, in_=yb_s[:, :], in_offset=bass.IndirectOffsetOnAxis(ap=DK[k][:, i:i + 1], axis=0),
                        bounds_check=creg(e), oob_is_err=False), [DK[k], ybJ], [Y], dma=True)
                    Ys.append(Y)
                pt = bank_rr.next()
                tr(pt[0:NE, 0:128], GT[:, i, :], identf[:], [GT.sub(i), identf], [pt])
                st["gT"] = gT = gT_r.next()
                cp("act", gT[:], pt[0:NE, 0:128], [pt], [gT])

            def g1():
                Ys = st["Ys"]; gT = st["gT"]
                st["acc"] = acc = acc_r.next()
                for hh in range(2):
                    pbd = bank_rr.next()
                    mm(pbd[:], gT[:], bd[:, hh * 512:(hh + 1) * 512], True, True, [gT, bd], [pbd])
                    stt(acc[:, hh * 512:(hh + 1) * 512], Ys[0][:, hh * 512:(hh + 1) * 512], GK[0][:, i:i + 1], pbd[:], ALU.mult, ALU.add, [Ys[0], GK[0], pbd], [acc])
                for k in range(1, TOPK):
                    stt(acc[:], Ys[k][:], GK[k][:, i:i + 1], acc[:], ALU.mult, ALU.add, [Ys[k], GK[k], acc], [acc])

            def g2():
                acc = st["acc"]
                tt("dve", acc[:], acc[:], g5_row[b][:], ALU.mult, [acc, g5_row[b]], [acc])
                tt("pool", acc[:], acc[:], st["x1t"][:], ALU.add, [acc, st["x1t"]], [acc])
                st["ss"] = ss = ss_r.next(); sq = sq_r.next()
                memset("pool", ss[:], 0.0, [ss])
                act(sq[:], acc[:], AF.Square, [acc, ss], [sq, ss], accum=ss[:, 0:1])
                act(ss[:, 1:2], ss[:, 0:1], AF.Ln, [ss], [ss], scale=1.0 / D, bias=epsc[:, 0:1])
                act(ss[:, 2:3], ss[:, 1:2], AF.Exp, [ss], [ss], scale=-0.5)

            def g3():
                acc = st["acc"]; ss = st["ss"]
                stt(acc[:], acc[:], ss[:, 2:3], gfin_row[:], ALU.mult, ALU.mult, [acc, ss, gfin_row], [acc])
                dma("act", out_d[i * 128:(i + 1) * 128, :], acc[:], [acc], [])
            return [g0, g1, g2, g3]

        gt_ = [g_tile(i) for i in range(64)]
        for step in range(64 + 3):
            for k_ in range(4):
                ui = step - k_
                if 0 <= ui < 64:
                    gt_[ui][k_]()
        A.release(m_moe)

    if stage >= 5:
        barrier()
        phase_moe()
    else:
        zt_ = A.tile("zt_", [128, D], F32)
        memset("pool", zt_[:], 0.0, [zt_])
        dma("sp", out_d[0:128, :], zt_[:], [zt_], [])

    S.finalize()
    return nc, dbg_out, S.stats, A.peak


def make_in_maps(inputs):
    f = lambda a: np.ascontiguousarray(a, dtype=np.float32)
    shared = dict(
        w_mod=f(inputs["w_mod"][0]), b_mod=f(inputs["b_mod"][0]), g_mix=f(inputs["g_mix"][0]), w_in=f(inputs["w_in"][0]),
        lam_re=f(inputs["lam_re"][0]).reshape(64, 64), lam_im=f(inputs["lam_im"][0]).reshape(64, 64),
        log_dt=f(inputs["log_dt"][0]).reshape(1, 64), b_re=f(inputs["b_re"][0]), b_im=f(inputs["b_im"][0]),
        c_re=f(inputs["c_re"][0]).reshape(8, 8, 16, 64), c_im=f(inputs["c_im"][0]).reshape(8, 8, 16, 64),
        d_skip=f(inputs["d_skip"][0]), w_glu=f(inputs["w_glu"][0]), conv_w=f(inputs["conv_w"][0]), conv_b=f(inputs["conv_b"][0]),
        w_ssm_br=f(inputs["w_ssm_br"][0]), w_conv_br=f(inputs["w_conv_br"][0]), w_o=f(inputs["w_o"][0]), g_ffn=f(inputs["g_ffn"][0]),
        w_router=f(inputs["w_router"][0]), b_router=f(inputs["b_router"][0]).reshape(1, NE), w_gu=f(inputs["w_gu"][0]),
        b_gu=f(inputs["b_gu"][0]), w_down=f(inputs["w_down"][0]), b_down=f(inputs["b_down"][0]), g_final=f(inputs["g_final"]),
    )
    maps = []
    for c in range(NCORES):
        bs = slice(c * NBL, (c + 1) * NBL)
        m = dict(shared)
        m["x"] = f(inputs["x"][bs]).reshape(NTOK, D)
        m["ctx"] = f(inputs["ctx"][bs]).reshape(NBL * CTXL, D)
        m["cond3"] = f(np.concatenate([inputs["c"][bs], inputs["c_ctx"][None, :]], axis=0))
        maps.append(m)
    return maps


def kernel(**inputs):
    nc, _, _, _ = build_program()
    res = run_bass_kernel_spmd(nc, make_in_maps(inputs), core_ids=list(range(NCORES)))
    outs = [np.asarray(r["out"]).reshape(NBL, L, D) for r in res.results]
    return np.concatenate(outs, axis=0).astype(np.float32)
```
